# Optimizing a Trainium2 kernel written in Bass

```python
import jax
import jax.numpy as jnp
from jax import lax
import numpy as np

D_MODEL = 1024
BATCH = 4
SEQ = 8192
DEPTH = 1

CTX_LEN = 256
GRID_W = 64
CHUNK = 64
EPS = 1e-6
HG_DK = 128
HG_HEADS = D_MODEL // HG_DK
HG_DV = D_MODEL // HG_HEADS
HG_FDIM = HG_HEADS * HG_DK
HG_VDIM = HG_HEADS * HG_DV
ML_HEADS = 4
ML_DV = D_MODEL // ML_HEADS
ML_DK = ML_DV // 2
ML_QKDIM = ML_HEADS * ML_DK
ML_VDIM = ML_HEADS * ML_DV
CONV_W = 5
N_GROUPS = 4
EXPERTS_PER_GROUP = 8
N_EXPERTS = N_GROUPS * EXPERTS_PER_GROUP
TOP_K = 2
D_EXPERT = 512
SEG_SIZES = (HG_FDIM, HG_FDIM, HG_FDIM, HG_VDIM, HG_VDIM,
             ML_QKDIM, ML_QKDIM, ML_VDIM, 2 * ML_HEADS, 2 * ML_HEADS, ML_VDIM,
             D_MODEL, D_MODEL)
SEG_OFFSETS = tuple(int(o) for o in np.cumsum(SEG_SIZES))
D_IN = SEG_OFFSETS[-1]
ML_F_OFFSET = SEG_OFFSETS[8]

kernel_name = 'hybrid_hgrn2_mlstm_hier_moe'


def rms_norm(x, g):
    xf = x.astype(jnp.float32)
    y = xf * lax.rsqrt(jnp.mean(xf * xf, axis=-1, keepdims=True) + EPS)
    return (y * g.astype(jnp.float32)).astype(x.dtype)


def head_rms_norm(y, g, n_heads):
    b, t, ch = y.shape
    yf = y.astype(jnp.float32).reshape(b, t, n_heads, ch // n_heads)
    yf = yf * lax.rsqrt(jnp.mean(yf * yf, axis=-1, keepdims=True) + EPS)
    return (yf.reshape(b, t, ch) * g.astype(jnp.float32)).astype(y.dtype)


def modulation(cvec, w_ada, b_ada):
    m = jax.nn.silu(cvec) @ w_ada + b_ada
    return jnp.split(m, 6, axis=-1)


def to_heads(a, n_heads):
    b, t, ch = a.shape
    return a.reshape(b, t, n_heads, ch // n_heads).transpose(0, 2, 1, 3)


def from_heads(a):
    b, h, t, d = a.shape
    return a.transpose(0, 2, 1, 3).reshape(b, t, h * d)


def to_colmajor(a):
    b, t, ch = a.shape
    rows = t // GRID_W
    return a.reshape(b, rows, GRID_W, ch).transpose(0, 2, 1, 3).reshape(b, t, ch)


def from_colmajor(a):
    b, t, ch = a.shape
    rows = t // GRID_W
    return a.reshape(b, GRID_W, rows, ch).transpose(0, 2, 1, 3).reshape(b, t, ch)


def flip_t(a):
    return jnp.flip(a, axis=2)


def chunked(a):
    nc = a.shape[2] // CHUNK
    a = a.reshape(a.shape[:2] + (nc, CHUNK) + a.shape[3:])
    return jnp.moveaxis(a, 2, 0)


def unchunk(a):
    a = jnp.moveaxis(a, 0, 2)
    return a.reshape(a.shape[:2] + (a.shape[2] * a.shape[3],) + a.shape[4:])


def short_conv(a, w, b):
    ch = a.shape[-1]
    pad = CONV_W // 2
    y = lax.conv_general_dilated(a, w[:, None, :].astype(a.dtype), window_strides=(1,),
                                 padding=[(pad, pad)], dimension_numbers=('NWC', 'WIO', 'NWC'),
                                 feature_group_count=ch)
    return y + b


def gla_scan(q, k, v, logf, s0):
    mask = jnp.tril(jnp.ones((CHUNK, CHUNK), dtype=bool))

    def step(s, inp):
        qc, kc, vc, lc = inp
        b = jnp.cumsum(lc, axis=2)
        diff = b[:, :, :, None, :] - b[:, :, None, :, :]
        w = jnp.exp(jnp.where(mask[:, :, None], diff, -jnp.inf))
        a = jnp.einsum('bhtk,bhtsk,bhsk->bhts', qc, w, kc)
        o = a @ vc + jnp.einsum('bhtk,bhkv->bhtv', qc * jnp.exp(b), s)
        bl = b[:, :, -1]
        s = jnp.exp(bl)[..., None] * s + jnp.einsum('bhsk,bhsv->bhkv', kc * jnp.exp(bl[:, :, None] - b), vc)
        return s, o

    s, o = lax.scan(step, s0, (chunked(q), chunked(k), chunked(v), chunked(logf)))
    return unchunk(o), s


def mlstm_scan(q, k, v, li, lf, state):
    mask = jnp.tril(jnp.ones((CHUNK, CHUNK), dtype=bool))

    def step(carry, inp):
        c_st, n_st, m_st = carry
        qc, kc, vc, ic, fc = inp
        b = jnp.cumsum(fc, axis=-1)
        d = jnp.where(mask, b[..., :, None] - b[..., None, :] + ic[..., None, :], -jnp.inf)
        g = b + m_st[..., None]
        m_t = jnp.maximum(jnp.max(d, axis=-1), g)
        p = jnp.exp(d - m_t[..., None]) * jnp.einsum('bhtk,bhsk->bhts', qc, kc)
        inter = jnp.exp(g - m_t)
        num = p @ vc + inter[..., None] * jnp.einsum('bhtk,bhkv->bhtv', qc, c_st)
        den = jnp.sum(p, axis=-1) + inter * jnp.einsum('bhtk,bhk->bht', qc, n_st)
        h = num / jnp.maximum(jnp.abs(den), jnp.exp(-m_t))[..., None]
        bl = b[..., -1]
        lw = bl[..., None] - b + ic
        m_new = jnp.maximum(bl + m_st, jnp.max(lw, axis=-1))
        decay = jnp.exp(bl + m_st - m_new)
        wk = jnp.exp(lw - m_new[..., None])[..., None] * kc
        c_st = decay[..., None, None] * c_st + jnp.einsum('bhsk,bhsv->bhkv', wk, vc)
        n_st = decay[..., None] * n_st + jnp.sum(wk, axis=2)
        return (c_st, n_st, m_new), h

    state, h = lax.scan(step, state, (chunked(q), chunked(k), chunked(v), chunked(li), chunked(lf)))
    return unchunk(h), state


def hgrn_core(hq, hff, hfb, hi, lb, s_f, s_b):
    f32 = jnp.float32
    q = to_heads(jax.nn.silu(hq.astype(f32)), HG_HEADS)
    v = to_heads(hi.astype(f32), HG_HEADS)

    def gates(hf, lbd):
        z = hf.astype(f32)
        f = lbd + (1.0 - lbd) * jax.nn.sigmoid(z)
        k = (1.0 - lbd) * jax.nn.sigmoid(-z)
        return to_heads(k, HG_HEADS), to_heads(jnp.log(f), HG_HEADS)

    kf, lff = gates(hff, lb[0])
    kb, lfb = gates(hfb, lb[1])
    o_f, s_f = gla_scan(q, kf, v, lff, s_f)
    o_b, s_b = gla_scan(flip_t(q), flip_t(kb), flip_t(v), flip_t(lfb), s_b)
    o = from_heads(o_f + flip_t(o_b))
    return o.astype(hq.dtype), s_f, s_b


def mlstm_core(hq, hk, hv, hi, hf, conv_w, conv_b, st_f, st_b, grid):
    f32 = jnp.float32
    if grid:
        hq, hk, hv, hi, hf = [to_colmajor(a) for a in (hq, hk, hv, hi, hf)]
    qk = jax.nn.silu(short_conv(jnp.concatenate([hq, hk], axis=-1), conv_w, conv_b))
    q = to_heads(qk[..., :ML_QKDIM].astype(f32), ML_HEADS) * (ML_DK ** -0.5)
    k = to_heads(qk[..., ML_QKDIM:].astype(f32), ML_HEADS)
    v = to_heads(hv.astype(f32), ML_HEADS)
    li = hi.astype(f32).transpose(0, 2, 1)
    lf = jax.nn.log_sigmoid(hf.astype(f32)).transpose(0, 2, 1)
    h_f, st_f = mlstm_scan(q, k, v, li[:, :ML_HEADS], lf[:, :ML_HEADS], st_f)
    h_b, st_b = mlstm_scan(flip_t(q), flip_t(k), flip_t(v), flip_t(li[:, ML_HEADS:]),
                           flip_t(lf[:, ML_HEADS:]), st_b)
    h = from_heads(h_f + flip_t(h_b))
    if grid:
        h = from_colmajor(h)
    return h.astype(hq.dtype), st_f, st_b


def token_mixer(h_lat, h_ctx, w_in, b_in, lb, conv_w, conv_b, hg_norm_g, ml_norm_g,
                w_pa, w_pb, w_out, need_ctx):
    f32 = jnp.float32
    p_lat = jnp.split(h_lat @ w_in + b_in, SEG_OFFSETS[:-1], axis=-1)
    p_ctx = jnp.split(h_ctx @ w_in + b_in, SEG_OFFSETS[:-1], axis=-1)
    bsz = h_lat.shape[0]
    s0 = jnp.zeros((bsz, HG_HEADS, HG_DK, HG_DV), f32)
    m0 = (jnp.zeros((bsz, ML_HEADS, ML_DK, ML_DV), f32),
          jnp.zeros((bsz, ML_HEADS, ML_DK), f32),
          jnp.zeros((bsz, ML_HEADS), f32))
    o_ctx, hs_f, hs_b = hgrn_core(p_ctx[0], p_ctx[1], p_ctx[2], p_ctx[3], lb, s0, s0)
    o_lat, _, _ = hgrn_core(p_lat[0], p_lat[1], p_lat[2], p_lat[3], lb, hs_f, hs_b)
    r_ctx, ms_f, ms_b = mlstm_core(p_ctx[5], p_ctx[6], p_ctx[7], p_ctx[8], p_ctx[9],
                                   conv_w, conv_b, m0, m0, False)
    r_lat, _, _ = mlstm_core(p_lat[5], p_lat[6], p_lat[7], p_lat[8], p_lat[9],
                             conv_w, conv_b, ms_f, ms_b, True)

    def merge(parts, o, r):
        y_a = head_rms_norm(o, hg_norm_g, HG_HEADS) * jax.nn.silu(parts[4])
        y_b = head_rms_norm(r, ml_norm_g, ML_HEADS) * jax.nn.sigmoid(parts[10])
        y = jax.nn.sigmoid(parts[11]) * (y_a @ w_pa) + jax.nn.sigmoid(parts[12]) * (y_b @ w_pb)
        return y @ w_out

    out_lat = merge(p_lat, o_lat, r_lat)
    out_ctx = merge(p_ctx, o_ctx, r_ctx) if need_ctx else None
    return out_lat, out_ctx


def hier_moe(h, w_group, b_group, w_router, b_router, w_gate, w_up, w_down):
    bsz, t, d = h.shape
    tok = h.reshape(bsz * t, d)
    n_tok = tok.shape[0]
    g_logits = (tok @ w_group + b_group).astype(jnp.float32)
    g_idx = jnp.argmax(g_logits, axis=-1)
    g_w = jnp.max(jax.nn.softmax(g_logits, axis=-1), axis=-1, keepdims=True)
    e_logits = (tok @ w_router + b_router).astype(jnp.float32).reshape(n_tok, N_GROUPS, EXPERTS_PER_GROUP)
    e_logits = e_logits[jnp.arange(n_tok), g_idx]
    top_v, top_i = lax.top_k(e_logits, TOP_K)
    top_w = jax.nn.softmax(top_v, axis=-1) * g_w
    expert_id = g_idx[:, None] * EXPERTS_PER_GROUP + top_i
    combine = jnp.sum(jax.nn.one_hot(expert_id, N_EXPERTS, dtype=jnp.float32) * top_w[..., None],
                      axis=1).astype(h.dtype)
    out = jnp.zeros_like(tok)
    for e in range(N_EXPERTS):
        a = jax.nn.silu(tok @ w_gate[e]) * (tok @ w_up[e])
        out = out + combine[:, e:e + 1] * (a @ w_down[e])
    return out.reshape(bsz, t, d)


def setup_inputs(seed: int = 0) -> dict:
    key = jax.random.key(seed)
    ks = jax.random.split(key, 32)
    nrm = jax.random.normal
    f32 = jnp.float32
    b_in = 0.02 * nrm(ks[9], (DEPTH, D_IN), f32)
    forget_bias = jnp.tile(jnp.linspace(3.0, 6.0, ML_HEADS), 2)
    b_in = b_in.at[:, ML_F_OFFSET:ML_F_OFFSET + 2 * ML_HEADS].add(forget_bias)
    return {
        'x': nrm(ks[0], (BATCH, SEQ, D_MODEL), f32),
        'c': nrm(ks[1], (BATCH, D_MODEL), f32),
        'ctx': nrm(ks[2], (BATCH, CTX_LEN, D_MODEL), f32),
        'c_ctx': nrm(ks[3], (D_MODEL,), f32),
        'w_ada': 0.5 * D_MODEL ** -0.5 * nrm(ks[4], (DEPTH, D_MODEL, 6 * D_MODEL), f32),
        'b_ada': 0.02 * nrm(ks[5], (DEPTH, 6 * D_MODEL), f32),
        'norm1_g': 1.0 + 0.02 * nrm(ks[6], (DEPTH, D_MODEL), f32),
        'norm2_g': 1.0 + 0.02 * nrm(ks[7], (DEPTH, D_MODEL), f32),
        'w_in': D_MODEL ** -0.5 * nrm(ks[8], (DEPTH, D_MODEL, D_IN), f32),
        'b_in': b_in,
        'hg_lb_logits': 0.5 * nrm(ks[10], (2, DEPTH + 1, HG_FDIM), f32),
        'hg_norm_g': 1.0 + 0.02 * nrm(ks[11], (DEPTH, HG_VDIM), f32),
        'ml_conv_w': CONV_W ** -0.5 * nrm(ks[12], (DEPTH, CONV_W, 2 * ML_QKDIM), f32),
        'ml_conv_b': 0.02 * nrm(ks[13], (DEPTH, 2 * ML_QKDIM), f32),
        'ml_norm_g': 1.0 + 0.02 * nrm(ks[14], (DEPTH, ML_VDIM), f32),
        'w_branch_a': HG_VDIM ** -0.5 * nrm(ks[15], (DEPTH, HG_VDIM, D_MODEL), f32),
        'w_branch_b': ML_VDIM ** -0.5 * nrm(ks[16], (DEPTH, ML_VDIM, D_MODEL), f32),
        'w_out': D_MODEL ** -0.5 * nrm(ks[17], (DEPTH, D_MODEL, D_MODEL), f32),
        'w_group': D_MODEL ** -0.5 * nrm(ks[18], (DEPTH, D_MODEL, N_GROUPS), f32),
        'b_group': 0.01 * nrm(ks[19], (DEPTH, N_GROUPS), f32),
        'w_router': D_MODEL ** -0.5 * nrm(ks[20], (DEPTH, D_MODEL, N_EXPERTS), f32),
        'b_router': 0.01 * nrm(ks[21], (DEPTH, N_EXPERTS), f32),
        'w_gate': D_MODEL ** -0.5 * nrm(ks[22], (DEPTH, N_EXPERTS, D_MODEL, D_EXPERT), f32),
        'w_up': D_MODEL ** -0.5 * nrm(ks[23], (DEPTH, N_EXPERTS, D_MODEL, D_EXPERT), f32),
        'w_down': D_EXPERT ** -0.5 * nrm(ks[24], (DEPTH, N_EXPERTS, D_EXPERT, D_MODEL), f32),
        'final_norm_g': 1.0 + 0.02 * nrm(ks[25], (D_MODEL,), f32),
    }


def reference(x, c, ctx, c_ctx, w_ada, b_ada, norm1_g, norm2_g, w_in, b_in, hg_lb_logits,
              hg_norm_g, ml_conv_w, ml_conv_b, ml_norm_g, w_branch_a, w_branch_b, w_out,
              w_group, b_group, w_router, b_router, w_gate, w_up, w_down, final_norm_g):
    lb_all = jnp.cumsum(jax.nn.softmax(hg_lb_logits.astype(jnp.float32), axis=1), axis=1)
    for l in range(DEPTH):
        need_ctx = l < DEPTH - 1
        sh1, sc1, g1, sh2, sc2, g2 = [m[:, None, :] for m in modulation(c, w_ada[l], b_ada[l])]
        csh1, csc1, cg1, csh2, csc2, cg2 = modulation(c_ctx, w_ada[l], b_ada[l])
        h_lat = rms_norm(x, norm1_g[l]) * (1.0 + sc1) + sh1
        h_ctx = rms_norm(ctx, norm1_g[l]) * (1.0 + csc1) + csh1
        mix_lat, mix_ctx = token_mixer(h_lat, h_ctx, w_in[l], b_in[l], lb_all[:, l], ml_conv_w[l],
                                       ml_conv_b[l], hg_norm_g[l], ml_norm_g[l], w_branch_a[l],
                                       w_branch_b[l], w_out[l], need_ctx)
        x = x + g1 * mix_lat
        h2 = rms_norm(x, norm2_g[l]) * (1.0 + sc2) + sh2
        x = x + g2 * hier_moe(h2, w_group[l], b_group[l], w_router[l], b_router[l],
                              w_gate[l], w_up[l], w_down[l])
        if need_ctx:
            ctx = ctx + cg1 * mix_ctx
            hc2 = rms_norm(ctx, norm2_g[l]) * (1.0 + csc2) + csh2
            ctx = ctx + cg2 * hier_moe(hc2, w_group[l], b_group[l], w_router[l], b_router[l],
                                       w_gate[l], w_up[l], w_down[l])
    return rms_norm(x, final_norm_g)
```

```python
import numpy as np
import concourse.bass as bass
import concourse.mybir as mybir
from concourse.bass_utils import run_bass_kernel_spmd

F32 = mybir.dt.float32
BF16 = mybir.dt.bfloat16
U8 = mybir.dt.uint8
AF = mybir.ActivationFunctionType
ALU = mybir.AluOpType
AX = mybir.AxisListType

D = 1024
import os as _os
T = int(_os.environ.get('KDBG_T', 8192))
TO = T // 2
GW = T // 128
CTX = 256
EPS = 1e-6
NE = 32
DE = 512
DIN = 10256
O_HQ, O_HF0, O_HF1, O_HI, O_HG = 0, 1024, 2048, 3072, 4096
O_MQ, O_MK, O_MV, O_MI, O_MF, O_MO, O_GA, O_GB = 5120, 5632, 6144, 7168, 7176, 7184, 8208, 9232


class Lane:
    def __init__(self, sem):
        self.sem = sem
        self.count = 0
        self.last = None


class Buf:
    __slots__ = ("name", "last_w", "readers")

    def __init__(self, name=""):
        self.name = name
        self.last_w = None
        self.readers = []


class TB:
    __slots__ = ("ap", "b")

    def __init__(self, ap, b=None):
        self.ap = ap
        self.b = b if b is not None else Buf()


class Op:
    __slots__ = ("eng", "fn", "deps", "lane", "lane_val", "needs_inc", "inc_idx", "is_dma")

    def __init__(self, eng, fn, lane):
        self.eng = eng
        self.fn = fn
        self.deps = set()
        self.lane = lane
        self.lane_val = 0
        self.needs_inc = False
        self.inc_idx = 0
        self.is_dma = lane is not None


class Sched:
    ENGS = ("pe", "act", "dve", "pool", "sp")

    def __init__(self, nc):
        self.nc = nc
        self.streams = {e: [] for e in self.ENGS}
        self.last_compute = {e: None for e in self.ENGS}
        self.lanes = []
        self.pending = {e: None for e in self.ENGS}

    def new_lane(self, name):
        ln = Lane(self.nc.alloc_semaphore(name=name))
        self.lanes.append(ln)
        return ln

    def barrier(self):
        deps = set()
        for e in self.ENGS:
            if self.last_compute[e] is not None:
                deps.add(self.last_compute[e])
        for ln in self.lanes:
            if ln.last is not None:
                deps.add(ln.last)
        for e in self.ENGS:
            prev = self.pending[e]
            self.pending[e] = set(deps) | (prev if prev else set())

    def op(self, eng, fn, reads=(), writes=(), lane=None):
        o = Op(eng, fn, lane)
        deps = set()
        for b in reads:
            if b.last_w is not None:
                deps.add(b.last_w)
        for b in writes:
            if b.last_w is not None:
                deps.add(b.last_w)
            deps.update(b.readers)
        if self.pending[eng]:
            deps.update(self.pending[eng])
            self.pending[eng] = None
        for b in reads:
            b.readers.append(o)
        for b in writes:
            b.last_w = o
            b.readers = []
        deps.discard(o)
        for d in deps:
            if (not o.is_dma) and (not d.is_dma) and o.eng == "pe" and d.eng == "pe":
                continue
            o.deps.add(d)
            if not d.is_dma:
                d.needs_inc = True
        if lane is not None:
            lane.count += 1
            o.lane_val = 16 * lane.count
            lane.last = o
        else:
            self.last_compute[eng] = o
        self.streams[eng].append(o)
        return o

    def emit(self, block, eng_sems, final_waits=()):
        nc = self.nc
        for e in self.ENGS:
            c = 0
            for o in self.streams[e]:
                if o.needs_inc and not o.is_dma:
                    c += 1
                    o.inc_idx = c
        deco = {"pe": block.tensor, "act": block.scalar, "dve": block.vector, "pool": block.gpsimd,
                "sp": block.sync}

        def make(e):
            def body(eng):
                waited = {}
                for o in self.streams[e]:
                    need = {}
                    for d in o.deps:
                        if d.is_dma:
                            s, v = d.lane.sem, d.lane_val
                        else:
                            s, v = eng_sems[d.eng], d.inc_idx
                        k = id(s)
                        if v > waited.get(k, 0) and v > need.get(k, (None, 0))[1]:
                            need[k] = (s, v)
                    for k, (s, v) in need.items():
                        eng.wait_ge(s, v)
                        waited[k] = v
                    ins = o.fn(eng)
                    if o.is_dma:
                        ins.then_inc(o.lane.sem, 16)
                    elif o.needs_inc:
                        ins.then_inc(eng_sems[e], 1)
                if e == "sp":
                    for d in final_waits:
                        eng.wait_ge(d.lane.sem, d.lane_val)

            return body

        for e in self.ENGS:
            deco[e](make(e))


def _dsz(dt):
    return 4 if dt == F32 else (2 if dt == BF16 else 1)


class Arena:
    def __init__(self, nc, nbytes):
        self.t = nc.alloc_sbuf_tensor("arena", [128, nbytes], U8)
        self.cap = nbytes
        self.off = 0

    def mark(self):
        return self.off

    def reset(self, m):
        self.off = m

    def alloc(self, free, dtype, parts=128):
        free = tuple(int(f) for f in free)
        n = int(np.prod(free)) * _dsz(dtype)
        assert self.off + n <= self.cap, f"SBUF arena overflow {self.off}+{n}>{self.cap}"
        v = self.t[0:parts, self.off:self.off + n].bitcast(dtype)
        self.off += (n + 63) // 64 * 64
        if len(free) == 2:
            v = v.rearrange("p (a b) -> p a b", a=free[0])
        elif len(free) == 3:
            v = v.rearrange("p (a b c) -> p a b c", a=free[0], b=free[1])
        return TB(v)


def build_program(stop_after=None, debug=False):
    nc = bass.Bass("TRN2", target_bir_lowering=False)
    S = Sched(nc)

    def din(name, shape):
        return nc.dram_tensor(name, list(shape), F32, kind="ExternalInput").ap()

    def dscr(name, shape, dt):
        kind = "ExternalOutput" if debug else "Internal"
        return nc.dram_tensor(name, list(shape), dt, kind=kind).ap()

    xv = din("xv", (T, D))
    ctxv = din("ctxv", (CTX, D))
    cvec = din("cvec", (128, 16))
    w_ada = din("w_ada", (D, 6 * D))
    b_ada = din("b_ada", (1, 6 * D))
    n1g = din("n1g", (1, D))
    n2g = din("n2g", (1, D))
    fing = din("fing", (1, D))
    w_in = din("w_in", (D, DIN))
    b_in = din("b_in", (1, DIN))
    lbl = din("lbl", (4, D))
    hgn = din("hgn", (1, D))
    mln = din("mln", (1, D))
    convw = din("convw", (5, D))
    convb = din("convb", (1, D))
    w_pa = din("w_pa", (D, D))
    w_pb = din("w_pb", (D, D))
    w_out = din("w_out", (D, D))
    w_rt = din("w_rt", (D, 36))
    b_rt = din("b_rt", (1, 36))
    NEd = NE if stop_after in (None, 'moe') else 1
    w_gate = din("w_gate", (NEd, D, DE))
    w_up = din("w_up", (NEd, D, DE))
    w_down = din("w_down", (NEd, DE, D))
    out = nc.dram_tensor("out", [TO, D], F32, kind="ExternalOutput").ap()

    xnT_r = dscr("xnT_r", (128, 8, T), BF16)
    xnT_c = dscr("xnT_c", (128, 8, T), BF16)
    xnT_x = dscr("xnT_x", (128, 8, CTX), BF16)
    scr_rows = dscr("scr_rows", (1, 8 * D), F32)
    o0_d = dscr("o0_d", (TO, D), BF16)
    ya_d = dscr("ya_d", (TO, D), BF16)
    yb_d = dscr("yb_d", (TO, D), BF16)
    xmid_d = dscr("xmid_d", (TO, D), F32)
    h2T_d = dscr("h2T_d", (128, 8, TO), BF16)
    ys_d = nc.dram_tensor("ys_d", [T // 256 + 1, 128, 8, 256], BF16, kind="Internal").ap()
    v_d = nc.dram_tensor("v_d", [T // 256 + 1, 2, 128, D], BF16, kind="Internal").ap()

    A = Arena(nc, 196000)
    PS = nc.alloc_psum_tensor("ps", [128, 8, 512], F32)

    def psf(bank, parts=128, n=512):
        return PS[0:parts, bank, 0:n]

    def psb(bank, parts=128):
        return PS[0:parts, bank, :].bitcast(BF16)

    psbuf = [Buf(f"psum{i}") for i in range(8)]

    eng_sems = {e: nc.alloc_semaphore(name=f"sem_{e}") for e in Sched.ENGS}
    L_setup = S.new_lane("setup")

    def dma(eng, out_ap, in_ap, lane, reads=(), writes=()):
        return S.op(eng, lambda e, o=out_ap, i=in_ap: e.dma_start(out=o, in_=i), reads, writes, lane=lane)

    def dma_small(out_ap, in_ap, reads=(), writes=()):
        def f(e, o=out_ap, i=in_ap):
            with nc.allow_non_contiguous_dma(reason="small setup vectors"):
                return e.dma_start(out=o, in_=i)
        return S.op("sp", f, reads, writes, lane=L_setup)

    ident_b = A.alloc((128,), BF16)
    ident_f = A.alloc((128,), F32)
    mask0 = A.alloc((128,), BF16)
    mask1 = A.alloc((128,), BF16)
    rmask = A.alloc((256,), F32)
    ones_b = A.alloc((8,), BF16)
    comb_all = A.alloc((32, 32), F32)
    eps_c = A.alloc((1,), F32)

    def memset(eng, t, ap, val):
        return S.op(eng, lambda e, a=ap, v=val: e.memset(a, v), (), (t.b,))

    def aff(t, ap_out, ap_in, pattern, cmp, fill, base, cm):
        return S.op("pool", lambda e: e.affine_select(out=ap_out, in_=ap_in, pattern=pattern, compare_op=cmp,
                                                      fill=fill, base=base, channel_multiplier=cm),
                    (t.b,), (t.b,))

    memset("pool", ident_b, ident_b.ap, 0.0)
    aff(ident_b, ident_b.ap, ident_b.ap, [[-1, 128]], ALU.not_equal, 1.0, 0, 1)
    memset("pool", ident_f, ident_f.ap, 0.0)
    aff(ident_f, ident_f.ap, ident_f.ap, [[-1, 128]], ALU.not_equal, 1.0, 0, 1)
    memset("pool", mask0, mask0.ap, 1.0)
    aff(mask0, mask0.ap, mask0.ap, [[1, 128]], ALU.is_ge, 0.0, 0, -1)
    memset("pool", mask0, mask0.ap[0:64, 64:128], 0.0)
    memset("pool", mask1, mask1.ap, 1.0)
    aff(mask1, mask1.ap, mask1.ap, [[-1, 128]], ALU.is_ge, 0.0, 0, 1)
    memset("pool", mask1, mask1.ap[64:128, 0:64], 0.0)
    memset("pool", rmask, rmask.ap, 1.0)
    memset("pool", rmask, rmask.ap[:, 0:256:64], 0.0)
    memset("pool", ones_b, ones_b.ap, 1.0)
    memset("pool", eps_c, eps_c.ap, EPS)
    MASKS = (mask0, mask1)

    base_mark = A.mark()

    def rstd_ops(ss, sd, rs, n, parts=128):
        S.op("act", lambda e: e.activation(out=sd.ap, in_=ss.ap, func=AF.Sqrt, bias=eps_c.ap[0:parts, :],
                                           scale=1.0 / n), (ss.b, eps_c.b), (sd.b,))
        S.op("dve", lambda e: e.reciprocal(out=rs.ap, in_=sd.ap), (sd.b,), (rs.b,))

    def phase_mod():
        c_sb = A.alloc((16,), F32)
        sil = A.alloc((16,), F32)
        bada = A.alloc((6 * D,), F32, parts=1)
        n1row = A.alloc((D,), F32, parts=1)
        n2row = A.alloc((D,), F32, parts=1)
        modr = A.alloc((6 * D,), F32, parts=1)
        modx = A.alloc((2 * D,), F32, parts=1)
        wblk = [A.alloc((8, 512), F32) for _ in range(2)]
        lw = [S.new_lane(f"wada{i}") for i in range(2)]
        dma_small(c_sb.ap, cvec, (), (c_sb.b,))
        dma_small(bada.ap, b_ada, (), (bada.b,))
        dma_small(n1row.ap, n1g, (), (n1row.b,))
        dma_small(n2row.ap, n2g, (), (n2row.b,))
        S.barrier()
        S.op("act", lambda e: e.activation(out=sil.ap, in_=c_sb.ap, func=AF.Silu), (c_sb.b,), (sil.b,))
        for j in range(12):
            wb = wblk[j % 2]
            dma("sp", wb.ap, w_ada[:, j * 512:(j + 1) * 512].rearrange("(k p) n -> p k n", p=128), lw[j % 2],
                (), (wb.b,))
            ba, bb = (0, 1) if j % 2 == 0 else (2, 3)

            def mmf(e, wb=wb, bank=ba, col=0):
                for k in range(8):
                    i = e.matmul(psf(bank, 1), lhsT=sil.ap[:, col + k:col + k + 1], rhs=wb.ap[:, k, :],
                                 start=(k == 0), stop=(k == 7))
                return i
            S.op("pe", mmf, (sil.b, wb.b), (psbuf[ba],))
            S.op("dve", lambda e, bank=ba, j=j: e.tensor_tensor(out=modr.ap[:, j * 512:(j + 1) * 512],
                                                                in0=psf(bank, 1),
                                                                in1=bada.ap[:, j * 512:(j + 1) * 512], op=ALU.add),
                 (psbuf[ba], bada.b), (modr.b,))
            if j < 4:
                S.op("pe", lambda e, wb=wb, bank=bb: mmf(e, wb, bank, 8), (sil.b, wb.b), (psbuf[bb],))
                S.op("dve", lambda e, bank=bb, j=j: e.tensor_tensor(out=modx.ap[:, j * 512:(j + 1) * 512],
                                                                    in0=psf(bank, 1),
                                                                    in1=bada.ap[:, j * 512:(j + 1) * 512],
                                                                    op=ALU.add),
                     (psbuf[bb], bada.b), (modx.b,))
        mr = A.alloc((8 * D,), F32, parts=1)

        def stt(out_ap, in0, scalar, in1, op0, op1, r, w):
            S.op("dve", lambda e: e.scalar_tensor_tensor(out=out_ap, in0=in0, scalar=scalar, in1=in1, op0=op0,
                                                         op1=op1), r, w)
        stt(mr.ap[:, 0:D], modr.ap[:, D:2 * D], 1.0, n1row.ap, ALU.add, ALU.mult, (modr.b, n1row.b), (mr.b,))
        S.op("dve", lambda e: e.tensor_copy(out=mr.ap[:, D:2 * D], in_=modr.ap[:, 0:D]), (modr.b,), (mr.b,))
        stt(mr.ap[:, 2 * D:3 * D], modx.ap[:, D:2 * D], 1.0, n1row.ap, ALU.add, ALU.mult, (modx.b, n1row.b), (mr.b,))
        S.op("dve", lambda e: e.tensor_copy(out=mr.ap[:, 3 * D:4 * D], in_=modx.ap[:, 0:D]), (modx.b,), (mr.b,))
        stt(mr.ap[:, 4 * D:5 * D], modr.ap[:, 4 * D:5 * D], 1.0, n2row.ap, ALU.add, ALU.mult, (modr.b, n2row.b),
            (mr.b,))
        S.op("dve", lambda e: e.tensor_copy(out=mr.ap[:, 5 * D:6 * D], in_=modr.ap[:, 3 * D:4 * D]), (modr.b,),
             (mr.b,))
        S.op("dve", lambda e: e.tensor_copy(out=mr.ap[:, 6 * D:7 * D], in_=modr.ap[:, 2 * D:3 * D]), (modr.b,),
             (mr.b,))
        S.op("dve", lambda e: e.tensor_copy(out=mr.ap[:, 7 * D:8 * D], in_=modr.ap[:, 5 * D:6 * D]), (modr.b,),
             (mr.b,))
        dma_small(scr_rows, mr.ap, (mr.b,), ())
        S.barrier()

    def bc_load(t, row_idx):
        src = scr_rows[0:1, row_idx * D:(row_idx + 1) * D].partition_broadcast(128)
        dma_small(t.ap.unsqueeze(1), src, (), (t.b,))

    def bc_load_in(t, dram_row_ap, n):
        src = dram_row_ap.partition_broadcast(128)
        dma_small(t.ap.unsqueeze(1), src, (), (t.b,))

    def pipeline_k(stages, items):
        K = len(stages)
        for t in range(len(items) + K - 1):
            for st_i in range(K):
                i = t - st_i
                if 0 <= i < len(items):
                    stages[st_i](items[i])

    def phase_norm():
        G = A.alloc((D,), F32)
        SH = A.alloc((D,), F32)
        Gx = A.alloc((D,), F32)
        SHx = A.alloc((D,), F32)
        bc_load(G, 0)
        bc_load(SH, 1)
        bc_load(Gx, 2)
        bc_load(SHx, 3)
        S.barrier()
        NBUF = 4
        xt = [A.alloc((D,), F32) for _ in range(NBUF)]
        lx = [S.new_lane(f"nx{i}") for i in range(NBUF)]
        junk = A.alloc((D,), BF16)
        ss = [A.alloc((1,), F32) for _ in range(NBUF)]
        sd = [A.alloc((1,), F32) for _ in range(NBUF)]
        rs = [A.alloc((1,), F32) for _ in range(NBUF)]
        tmp = [A.alloc((D,), F32) for _ in range(NBUF)]
        hb = [A.alloc((D,), BF16) for _ in range(NBUF)]
        stage = [A.alloc((8, 512), BF16) for _ in range(2)]
        lst = [S.new_lane(f"nst{i}") for i in range(2)]
        xcol = xv.rearrange("(r w) d -> w r d", w=GW)

        jobs = []
        for i in range(T // 128):
            jobs.append((xv[i * 128:(i + 1) * 128, :], G, SH, xnT_r, i * 128))
        for w in range(GW):
            jobs.append((xcol[w], G, SH, xnT_c, w * 128))
        for i in range(CTX // 128):
            jobs.append((ctxv[i * 128:(i + 1) * 128, :], Gx, SHx, xnT_x, i * 128))
        stt = {"nst": 0, "st": None, "lane": None, "off": 0, "cnt": 0}

        def front(n):
            src, g, sh, dst, off = jobs[n]
            a = n % NBUF
            x, s1, s2, r1, t1, h1 = xt[a], ss[a], sd[a], rs[a], tmp[a], hb[a]
            dma("sp", x.ap, src, lx[a], (), (x.b,))
            S.op("act", lambda e, x=x, s1=s1: e.activation(out=junk.ap, in_=x.ap, func=AF.Square,
                                                           accum_out=s1.ap), (x.b,), (junk.b, s1.b))
            rstd_ops(s1, s2, r1, D)
            S.op("dve", lambda e, x=x, r1=r1, g=g, t1=t1: e.scalar_tensor_tensor(
                out=t1.ap, in0=x.ap, scalar=r1.ap, in1=g.ap, op0=ALU.mult, op1=ALU.mult),
                (x.b, r1.b, g.b), (t1.b,))
            S.op("pool", lambda e, t1=t1, sh=sh, h1=h1: e.tensor_tensor(out=h1.ap, in0=t1.ap, in1=sh.ap,
                                                                         op=ALU.add), (t1.b, sh.b), (h1.b,))
            bank = n % 2

            def tr(e, h1=h1, bank=bank):
                pv = psb(bank).rearrange("p (k t) -> p k t", k=8)
                for k in range(8):
                    i = e.transpose(out=pv[:, k, :], in_=h1.ap[:, k * 128:(k + 1) * 128], identity=ident_b.ap)
                return i
            S.op("pe", tr, (h1.b, ident_b.b), (psbuf[bank],))

        def back(n):
            src, g, sh, dst, off = jobs[n]
            bank = n % 2
            first = (n == 0) or (jobs[n - 1][3] is not dst) or (off % 512 == 0)
            if first:
                stt["st"] = stage[stt["nst"] % 2]
                stt["lane"] = lst[stt["nst"] % 2]
                stt["nst"] += 1
                stt["off"] = off
                stt["cnt"] = 0
            st = stt["st"]
            S.op("act", lambda e, st=st, c=stt["cnt"], bank=bank: e.activation(
                out=st.ap[:, :, c * 128:(c + 1) * 128], in_=psb(bank).rearrange("p (k t) -> p k t", k=8),
                func=AF.Copy), (psbuf[bank],), (st.b,))
            stt["cnt"] += 1
            last = (n == len(jobs) - 1) or (jobs[n + 1][3] is not dst) or ((off + 128) % 512 == 0)
            if last:
                dma("sp", dst[:, :, stt["off"]:stt["off"] + stt["cnt"] * 128], st.ap[:, :, 0:stt["cnt"] * 128],
                    stt["lane"], (st.b,), ())
        pipeline_k([front, back], list(range(len(jobs))))
        S.barrier()

    def pipeline(stages):
        prev = None
        for p, c in stages:
            if prev is None:
                for _ in p:
                    pass
            else:
                ga, gb = p, prev
                da = db = False
                while not (da and db):
                    if not da:
                        try:
                            next(ga)
                        except StopIteration:
                            da = True
                    if not db:
                        try:
                            next(gb)
                        except StopIteration:
                            db = True
            prev = c
        if prev is not None:
            for _ in prev:
                pass

    class ChainCfg:
        pass

    def make_chain(H, DV, with_den, name):
        C = ChainCfg()
        C.H, C.DV = H, DV
        C.S2 = [A.alloc((H, DV), F32) for _ in range(2)]
        C.cur = 0
        C.Spb = [A.alloc((H, DV), BF16) for _ in range(2)]
        C.ke_tok = [A.alloc((H, 128), BF16) for _ in range(2)]
        C.at_sb = [A.alloc((H, 128), BF16) for _ in range(2)]
        if with_den:
            C.n = A.alloc((H,), F32)
            C.np_ = A.alloc((H,), F32)
            C.npb = [A.alloc((H,), BF16) for _ in range(2)]
        memset("pool", C.S2[0], C.S2[0].ap, 0.0)
        if with_den:
            memset("pool", C.n, C.n.ap, 0.0)
        C.cnt = 0
        return C

    def chain_tile(C, d, qeT, keT, tsl, v_tok, E_end, ecol, chunks, R, with_den=False):
        H, DV = C.H, C.DV
        n = C.cnt
        C.cnt += 1
        ke_tok = C.ke_tok[n % 2]
        at_sb = C.at_sb[n % 2]
        hpb = 512 // DV

        def tr(e):
            pv = psb(2).rearrange("p (h k) -> p h k", h=8)
            for h in range(H):
                i = e.transpose(out=pv[:, h, :], in_=keT.ap[:, h, tsl], identity=ident_b.ap)
            return i
        S.op("pe", tr, (keT.b, ident_b.b), (psbuf[2],))
        S.op("act", lambda e: e.activation(out=ke_tok.ap, in_=psb(2).rearrange("p (h k) -> p h k", h=8)[:, 0:H, :],
                                           func=AF.Copy), (psbuf[2],), (ke_tok.b,))
        t0 = tsl.start
        if R > 0:
            if H <= 4:
                def at(e):
                    pv = psf(3).rearrange("p (h t) -> p h t", h=4)
                    for h in range(H):
                        i = e.matmul(pv[0:R, h, 0:R], lhsT=keT.ap[:, h, t0:t0 + R],
                                     rhs=qeT.ap[:, h, t0:t0 + R], start=True, stop=True)
                    return i
                S.op("pe", at, (keT.b, qeT.b), (psbuf[3],))
                S.op("dve", lambda e: e.tensor_tensor(
                    out=at_sb.ap[0:R, :, 0:R], in0=psf(3).rearrange("p (h t) -> p h t", h=4)[0:R, 0:H, 0:R],
                    in1=MASKS[d].ap[0:R, 0:R].unsqueeze(1).broadcast_to([R, H, R]), op=ALU.mult),
                    (psbuf[3], MASKS[d].b), (at_sb.b,))
            else:
                for half in range(2):
                    def at2(e, half=half):
                        pv = psf(3).rearrange("p (h t) -> p h t", h=4)
                        for hh in range(4):
                            h = half * 4 + hh
                            i = e.matmul(pv[0:R, hh, 0:R], lhsT=keT.ap[:, h, t0:t0 + R],
                                         rhs=qeT.ap[:, h, t0:t0 + R], start=True, stop=True)
                        return i
                    S.op("pe", at2, (keT.b, qeT.b), (psbuf[3],))
                    S.op("dve", lambda e, half=half: e.tensor_tensor(
                        out=at_sb.ap[0:R, half * 4:half * 4 + 4, 0:R],
                        in0=psf(3).rearrange("p (h t) -> p h t", h=4)[0:R, :, 0:R],
                        in1=MASKS[d].ap[0:R, 0:R].unsqueeze(1).broadcast_to([R, 4, R]), op=ALU.mult),
                        (psbuf[3], MASKS[d].b), (at_sb.b,))
        yield
        spb_used = {}
        for ci, (lo, outf) in enumerate(chunks):
            ec = ecol[lo]
            Spb = C.Spb[ci % 2]
            Sc = C.S2[C.cur]
            Sn = C.S2[1 - C.cur]
            C.cur = 1 - C.cur
            if outf:
                S.op("pool", lambda e, ec=ec, Sc=Sc, Spb=Spb: e.tensor_tensor(
                    out=Spb.ap, in0=Sc.ap, in1=E_end.ap[:, :, ec:ec + 1].broadcast_to([128, H, DV]), op=ALU.mult),
                    (Sc.b, E_end.b), (Spb.b,))
                spb_used[lo] = Spb

            def kv(e, lo=lo):
                for h in range(H):
                    bank = 4 + h // hpb
                    col = (h % hpb) * DV
                    i = e.matmul(PS[:, bank, col:col + DV], lhsT=ke_tok.ap[lo:lo + 64, h, :],
                                 rhs=v_tok.ap[lo:lo + 64, h * DV:(h + 1) * DV], start=True, stop=True)
                return i
            S.op("pe", kv, (ke_tok.b, v_tok.b), (psbuf[4], psbuf[5]))

            def su(e, ec=ec, Sc=Sc, Sn=Sn):
                for h in range(H):
                    bank = 4 + h // hpb
                    col = (h % hpb) * DV
                    i = e.scalar_tensor_tensor(out=Sn.ap[:, h, :], in0=Sc.ap[:, h, :], scalar=E_end.ap[:, h, ec:ec + 1],
                                               in1=PS[:, bank, col:col + DV], op0=ALU.mult, op1=ALU.add)
                return i
            S.op("dve", su, (Sc.b, E_end.b, psbuf[4], psbuf[5]), (Sn.b,))
            if with_den:
                npb = C.npb[ci % 2]
                S.op("dve", lambda e, ec=ec: e.tensor_tensor(out=C.np_.ap, in0=C.n.ap, in1=E_end.ap[:, :, ec],
                                                            op=ALU.mult), (C.n.b, E_end.b), (C.np_.b,))
                if outf:
                    S.op("act", lambda e, npb=npb: e.activation(out=npb.ap, in_=C.np_.ap, func=AF.Copy),
                         (C.np_.b,), (npb.b,))
                    spb_used[("n", lo)] = npb

                def kn(e, lo=lo):
                    for h in range(H):
                        i = e.matmul(PS[:, 3, 64 + h:65 + h], lhsT=ke_tok.ap[lo:lo + 64, h, :],
                                     rhs=ones_b.ap[lo:lo + 64, 0:1], start=True, stop=True)
                    return i
                S.op("pe", kn, (ke_tok.b, ones_b.b), (psbuf[3],))
                S.op("dve", lambda e: e.tensor_tensor(out=C.n.ap, in0=C.np_.ap, in1=PS[:, 3, 64:64 + H],
                                                      op=ALU.add), (C.np_.b, psbuf[3]), (C.n.b,))
            yield
        if R > 0:
            outs = [lo for (lo, outf) in chunks if outf]
            rd = [at_sb.b, v_tok.b, qeT.b] + [spb_used[lo].b for lo in outs]

            def om(e):
                for h in range(H):
                    bank = 6 + h // hpb
                    col = (h % hpb) * DV
                    e.matmul(PS[0:R, bank, col:col + DV], lhsT=at_sb.ap[0:R, h, 0:R],
                             rhs=v_tok.ap[0:R, h * DV:(h + 1) * DV], start=True, stop=False)
                    for j, lo in enumerate(outs):
                        i = e.matmul(PS[lo:lo + 64, bank, col:col + DV], lhsT=qeT.ap[:, h, t0 + lo:t0 + lo + 64],
                                     rhs=spb_used[lo].ap[:, h, :], start=False, stop=(j == len(outs) - 1))
                return i
            S.op("pe", om, rd, (psbuf[6], psbuf[7]))
            if with_den:
                rd2 = [at_sb.b, ones_b.b, qeT.b] + [spb_used[("n", lo)].b for lo in outs]

                def dn(e):
                    for h in range(H):
                        e.matmul(PS[0:R, 3, 192 + h:193 + h], lhsT=at_sb.ap[0:R, h, 0:R], rhs=ones_b.ap[0:R, 0:1],
                                 start=True, stop=True)
                        for lo in outs:
                            i = e.matmul(PS[lo:lo + 64, 3, 320 + h:321 + h], lhsT=qeT.ap[:, h, t0 + lo:t0 + lo + 64],
                                         rhs=spb_used[("n", lo)].ap[:, h:h + 1], start=True, stop=True)
                    return i
                S.op("pe", dn, rd2, (psbuf[3],))
        yield

    def load_w(t, col0, ncols, lane, eng="pool"):
        return dma(eng, t.ap, w_in[:, col0:col0 + ncols].rearrange("(k p) n -> p k n", p=128), lane, (), (t.b,))

    def phase_hgrn(d):
        m0 = A.mark()
        Wq = A.alloc((8, D), BF16)
        Wf = A.alloc((8, D), BF16)
        Wi = A.alloc((8, D), BF16)
        lws = [S.new_lane(f"hw{d}{i}") for i in range(4)]
        load_w(Wq, O_HQ, D, lws[0])
        load_w(Wf, O_HF0 if d == 0 else O_HF1, D, lws[1])
        load_w(Wi, O_HI, D, lws[2])
        if d == 1:
            Wg = A.alloc((8, D), BF16)
            load_w(Wg, O_HG, D, lws[3])
            bg_bc = A.alloc((D,), F32)
            bc_load_in(bg_bc, b_in[0:1, O_HG:O_HG + D], D)
            hgn_bc = A.alloc((D,), F32)
            bc_load_in(hgn_bc, hgn[0:1, :], D)
        bi_bc = A.alloc((D,), F32)
        bc_load_in(bi_bc, b_in[0:1, O_HI:O_HI + D], D)
        bq = A.alloc((8,), F32)
        bf = A.alloc((8,), F32)
        l0 = A.alloc((8,), F32)
        l1 = A.alloc((8,), F32)
        lb = A.alloc((8,), F32)
        oml = A.alloc((8,), F32)
        noml = A.alloc((8,), F32)
        fo = O_HF0 if d == 0 else O_HF1
        dma_small(bq.ap, b_in[0, O_HQ:O_HQ + D].rearrange("(h p) -> p h", p=128), (), (bq.b,))
        dma_small(bf.ap, b_in[0, fo:fo + D].rearrange("(h p) -> p h", p=128), (), (bf.b,))
        dma_small(l0.ap, lbl[2 * d, :].rearrange("(h p) -> p h", p=128), (), (l0.b,))
        dma_small(l1.ap, lbl[2 * d + 1, :].rearrange("(h p) -> p h", p=128), (), (l1.b,))
        S.barrier()
        S.op("dve", lambda e: e.tensor_tensor(out=l0.ap, in0=l0.ap, in1=l1.ap, op=ALU.subtract), (l0.b, l1.b),
             (l0.b,))
        S.op("act", lambda e: e.activation(out=lb.ap, in_=l0.ap, func=AF.Sigmoid), (l0.b,), (lb.b,))
        S.op("dve", lambda e: e.tensor_scalar(out=oml.ap, in0=lb.ap, scalar1=-1.0, scalar2=1.0, op0=ALU.mult,
                                              op1=ALU.add), (lb.b,), (oml.b,))
        S.op("dve", lambda e: e.tensor_scalar(out=noml.ap, in0=oml.ap, scalar1=-1.0, scalar2=None, op0=ALU.mult),
             (oml.b,), (noml.b,))

        NB = 256
        xblk = [A.alloc((8, NB), BF16) for _ in range(2)]
        lxb = [S.new_lane(f"hx{d}{i}") for i in range(2)]
        X1 = A.alloc((8, NB), F32)
        X2 = A.alloc((8, NB), F32)
        X3 = A.alloc((8, NB), F32)
        QS = A.alloc((8, NB), BF16)
        qeT = [A.alloc((8, NB), BF16) for _ in range(2)]
        keT = [A.alloc((8, NB), BF16) for _ in range(2)]
        v_tok = [A.alloc((D,), BF16) for _ in range(4)]
        E_end = [A.alloc((8, 4), F32) for _ in range(2)]
        C = make_chain(8, 128, False, f"hg{d}")
        if d == 0:
            o0s = [A.alloc((D,), BF16) for _ in range(2)]
            lo0 = [S.new_lane(f"ho0{i}") for i in range(2)]
        else:
            gate = [A.alloc((D,), BF16) for _ in range(4)]
            o0t = [A.alloc((D,), BF16) for _ in range(2)]
            lo0 = [S.new_lane(f"ho1{i}") for i in range(2)]
            osum = A.alloc((D,), F32)
            osq = A.alloc((D,), F32)
            gtmp = osq
            ss8 = A.alloc((8,), F32)
            sd8 = A.alloc((8,), F32)
            rs8 = A.alloc((8,), F32)
            yat = [A.alloc((D,), BF16) for _ in range(2)]
            lya = [S.new_lane(f"hya{i}") for i in range(2)]

        if d == 0:
            blocks = [("x", 0, False)] + [("r", 256 * i, True) for i in range(TO // NB)]
        else:
            blocks = [("x", 0, False)] + [("r", TO + 256 * i, False) for i in reversed(range(TO // NB))] + \
                     [("r", 256 * i, True) for i in reversed(range(TO // NB))]
        tiles = [0, 1] if d == 0 else [1, 0]
        cnt = [0]

        def prep(bn, src, tok0, outf):
            xb = xblk[bn % 2]
            srcap = xnT_x[:, :, 0:NB] if src == "x" else xnT_r[:, :, tok0:tok0 + NB]
            dma("sp", xb.ap, srcap, lxb[bn % 2], (), (xb.b,))
            qe, ke, Ee = qeT[bn % 2], keT[bn % 2], E_end[bn % 2]
            for h in range(8):
                bank = h % 2

                def fm(e, h=h, bank=bank, xb=xb):
                    for k in range(8):
                        i = e.matmul(psf(bank, 128, NB), lhsT=Wf.ap[:, k, h * 128:(h + 1) * 128], rhs=xb.ap[:, k, :],
                                     start=(k == 0), stop=(k == 7))
                    return i
                S.op("pe", fm, (Wf.b, xb.b), (psbuf[bank],))
                S.op("act", lambda e, h=h, bank=bank: e.activation(out=X1.ap[:, h, :], in_=psf(bank, 128, NB),
                                                                   func=AF.Sigmoid, bias=bf.ap[:, h:h + 1]),
                     (psbuf[bank], bf.b), (X1.b,))
                yield
            for h in range(8):
                S.op("act", lambda e, h=h: e.activation(out=X2.ap[:, h, :], in_=X1.ap[:, h, :], func=AF.Ln,
                                                        bias=lb.ap[:, h:h + 1], scale=oml.ap[:, h:h + 1]),
                     (X1.b, lb.b, oml.b), (X2.b,))
            yield
            for h in range(8):
                S.op("dve", lambda e, h=h: e.tensor_scalar(out=X1.ap[:, h, :], in0=X1.ap[:, h, :],
                                                           scalar1=noml.ap[:, h:h + 1], scalar2=oml.ap[:, h:h + 1],
                                                           op0=ALU.mult, op1=ALU.add), (X1.b, noml.b, oml.b),
                     (X1.b,))
            yield
            for h in range(8):
                S.op("dve", lambda e, h=h: e.tensor_tensor_scan(out=X3.ap[:, h, :], data0=rmask.ap[:, 0:NB],
                                                                data1=X2.ap[:, h, :], initial=0.0, op0=ALU.mult,
                                                                op1=ALU.add), (rmask.b, X2.b), (X3.b,))
            yield
            S.op("act", lambda e, Ee=Ee: e.activation(out=Ee.ap, in_=X3.ap[:, :, 63:NB:64], func=AF.Exp),
                 (X3.b,), (Ee.b,))
            if d == 0:
                S.op("dve", lambda e: e.tensor_tensor(
                    out=X2.ap.rearrange("p h (c t) -> p h c t", t=64),
                    in0=X3.ap[:, :, 63:NB:64].unsqueeze(3).broadcast_to([128, 8, NB // 64, 64]),
                    in1=X3.ap.rearrange("p h (c t) -> p h c t", t=64), op=ALU.subtract), (X3.b,), (X2.b,))
            else:
                S.op("pool", lambda e: e.tensor_tensor(out=X2.ap, in0=X3.ap, in1=X2.ap, op=ALU.subtract),
                     (X3.b, X2.b), (X2.b,))
            S.op("act", lambda e: e.activation(out=X3.ap, in_=X2.ap, func=AF.Exp), (X2.b,), (X3.b,))
            S.op("pool", lambda e, ke=ke: e.tensor_tensor(out=ke.ap, in0=X1.ap, in1=X3.ap, op=ALU.mult),
                 (X1.b, X3.b), (ke.b,))
            yield
            if outf:
                S.op("act", lambda e: e.activation(out=X2.ap, in_=X2.ap, func=AF.Exp, scale=-1.0), (X2.b,),
                     (X2.b,))
                for h in range(8):
                    bank = h % 2

                    def qm(e, h=h, bank=bank, xb=xb):
                        for k in range(8):
                            i = e.matmul(psf(bank, 128, NB), lhsT=Wq.ap[:, k, h * 128:(h + 1) * 128],
                                         rhs=xb.ap[:, k, :], start=(k == 0), stop=(k == 7))
                        return i
                    S.op("pe", qm, (Wq.b, xb.b), (psbuf[bank],))
                    S.op("act", lambda e, h=h, bank=bank: e.activation(out=QS.ap[:, h, :], in_=psf(bank, 128, NB),
                                                                       func=AF.Silu, bias=bq.ap[:, h:h + 1]),
                         (psbuf[bank], bq.b), (QS.b,))
                    yield
                S.op("dve", lambda e, qe=qe: e.tensor_tensor(out=qe.ap, in0=QS.ap, in1=X2.ap, op=ALU.mult),
                     (QS.b, X2.b), (qe.b,))
                yield
            for ti in tiles:
                vt = v_tok[(bn % 2) * 2 + ti]
                for half in range(2):
                    bank = half

                    def vm(e, ti=ti, half=half, bank=bank, xb=xb):
                        for k in range(8):
                            i = e.matmul(psf(bank), lhsT=xb.ap[:, k, ti * 128:(ti + 1) * 128],
                                         rhs=Wi.ap[:, k, half * 512:(half + 1) * 512], start=(k == 0), stop=(k == 7))
                        return i
                    S.op("pe", vm, (Wi.b, xb.b), (psbuf[bank],))
                    S.op("dve", lambda e, vt=vt, half=half, bank=bank: e.tensor_tensor(
                        out=vt.ap[:, half * 512:(half + 1) * 512], in0=psf(bank),
                        in1=bi_bc.ap[:, half * 512:(half + 1) * 512], op=ALU.add), (psbuf[bank], bi_bc.b), (vt.b,))
                yield
                if d == 1 and outf:
                    gt = gate[(bn % 2) * 2 + ti]
                    for half in range(2):
                        bank = half

                        def gm(e, ti=ti, half=half, bank=bank, xb=xb):
                            for k in range(8):
                                i = e.matmul(psf(bank), lhsT=xb.ap[:, k, ti * 128:(ti + 1) * 128],
                                             rhs=Wg.ap[:, k, half * 512:(half + 1) * 512], start=(k == 0),
                                             stop=(k == 7))
                            return i
                        S.op("pe", gm, (Wg.b, xb.b), (psbuf[bank],))
                        S.op("dve", lambda e, half=half, bank=bank: e.tensor_tensor(
                            out=gtmp.ap[:, half * 512:(half + 1) * 512], in0=psf(bank),
                            in1=bg_bc.ap[:, half * 512:(half + 1) * 512], op=ALU.add), (psbuf[bank], bg_bc.b),
                            (gtmp.b,))
                    S.op("act", lambda e, gt=gt: e.activation(out=gt.ap, in_=gtmp.ap, func=AF.Silu), (gtmp.b,),
                         (gt.b,))
                    yield

        def chain(bn, src, tok0, outf):
            qe, ke, Ee = qeT[bn % 2], keT[bn % 2], E_end[bn % 2]
            for ti in tiles:
                vt = v_tok[(bn % 2) * 2 + ti]
                chunks = [(0, outf), (64, outf)] if d == 0 else [(64, outf), (0, outf)]
                ecol = {0: 2 * ti, 64: 2 * ti + 1}
                yield from chain_tile(C, d, qe, ke, slice(ti * 128, (ti + 1) * 128), vt, Ee, ecol, chunks,
                                      128 if outf else 0)
                if outf:
                    vt_n = cnt[0]
                    cnt[0] += 1
                    gtile = (tok0 + ti * 128) // 128
                    ops = PS[:, 6:8, :].rearrange("p b n -> p (b n)")
                    if d == 0:
                        ot = o0s[vt_n % 2]
                        S.op("act", lambda e, ot=ot: e.activation(out=ot.ap, in_=ops, func=AF.Copy),
                             (psbuf[6], psbuf[7]), (ot.b,))
                        dma("sp", o0_d[gtile * 128:(gtile + 1) * 128, :], ot.ap, lo0[vt_n % 2], (ot.b,), ())
                    else:
                        gt = gate[(bn % 2) * 2 + ti]
                        ot = o0t[vt_n % 2]
                        dma("sp", ot.ap, o0_d[gtile * 128:(gtile + 1) * 128, :], lo0[vt_n % 2], (), (ot.b,))
                        S.op("dve", lambda e, ot=ot: e.tensor_tensor(out=osum.ap, in0=ops, in1=ot.ap, op=ALU.add),
                             (psbuf[6], psbuf[7], ot.b), (osum.b,))
                        S.op("pool", lambda e: e.tensor_tensor(out=osq.ap, in0=osum.ap, in1=osum.ap, op=ALU.mult),
                             (osum.b,), (osq.b,))
                        S.op("dve", lambda e: e.tensor_reduce(out=ss8.ap, in_=osq.ap.rearrange("p (h v) -> p h v", h=8),
                                                              axis=AX.X, op=ALU.add), (osq.b,), (ss8.b,))
                        rstd_ops(ss8, sd8, rs8, 128)
                        yield
                        S.op("dve", lambda e: e.tensor_tensor(
                            out=osum.ap.rearrange("p (h v) -> p h v", h=8),
                            in0=osum.ap.rearrange("p (h v) -> p h v", h=8),
                            in1=rs8.ap.unsqueeze(2).broadcast_to([128, 8, 128]), op=ALU.mult), (osum.b, rs8.b),
                            (osum.b,))
                        S.op("pool", lambda e: e.tensor_tensor(out=osq.ap, in0=osum.ap, in1=hgn_bc.ap, op=ALU.mult),
                             (osum.b, hgn_bc.b), (osq.b,))
                        ya = yat[vt_n % 2]
                        S.op("dve", lambda e, ya=ya, gt=gt: e.tensor_tensor(out=ya.ap, in0=osq.ap, in1=gt.ap,
                                                                           op=ALU.mult), (osq.b, gt.b), (ya.b,))
                        dma("sp", ya_d[gtile * 128:(gtile + 1) * 128, :], ya.ap, lya[vt_n % 2], (ya.b,), ())
                    yield

        pipeline([(prep(bn, *blk), chain(bn, *blk)) for bn, blk in enumerate(blocks)])
        S.barrier()
        A.reset(m0)

    def phase_mlstm(d):
        m0 = A.mark()
        Wq = A.alloc((8, 512), BF16)
        Wk = A.alloc((8, 512), BF16)
        Wv = A.alloc((8, D), BF16)
        Wgt = A.alloc((8, 16), BF16)
        lws = [S.new_lane(f"mw{d}{i}") for i in range(5)]
        load_w(Wq, O_MQ, 512, lws[0])
        load_w(Wk, O_MK, 512, lws[1])
        load_w(Wv, O_MV, D, lws[2])
        load_w(Wgt, O_MI, 16, lws[3])
        if d == 1:
            Wo = A.alloc((8, D), BF16)
            load_w(Wo, O_MO, D, lws[4])
            bo_bc = A.alloc((D,), F32)
            bc_load_in(bo_bc, b_in[0:1, O_MO:O_MO + D], D)
            mln_bc = A.alloc((D,), F32)
            bc_load_in(mln_bc, mln[0:1, :], D)
        bv_bc = A.alloc((D,), F32)
        bc_load_in(bv_bc, b_in[0:1, O_MV:O_MV + D], D)
        bqk = A.alloc((8,), F32)
        cw = A.alloc((5, 8), F32)
        cb = A.alloc((8,), F32)
        bgt = A.alloc((1,), F32, parts=16)
        dma_small(bqk.ap, b_in[0, O_MQ:O_MQ + D].rearrange("(g p) -> p g", p=128), (), (bqk.b,))
        for j in range(5):
            dma_small(cw.ap[:, j, :], convw[j, :].rearrange("(g p) -> p g", p=128), (), (cw.b,))
        dma_small(cb.ap, convb[0, :].rearrange("(g p) -> p g", p=128), (), (cb.b,))
        dma_small(bgt.ap, b_in[0, O_MI:O_MI + 16].rearrange("(p o) -> p o", o=1), (), (bgt.b,))
        Eoh = A.alloc((16, 128), F32, parts=16)
        Selk = A.alloc((4, 128), F32, parts=16)
        Selq = A.alloc((4, 128), F32, parts=16)
        Self_ = A.alloc((4, 128), F32, parts=16)
        memset("pool", Eoh, Eoh.ap, 1.0)
        for J in range(16):
            aff(Eoh, Eoh.ap[:, J, :], Eoh.ap[:, J, :], [[0, 128]], ALU.is_equal, 0.0, -J, 1)
        for h in range(4):
            S.op("pool", lambda e, h=h: e.tensor_tensor(out=Selk.ap[:, h, :], in0=Eoh.ap[:, 4 * d + h, :],
                                                        in1=Eoh.ap[:, 8 + 4 * d + h, :], op=ALU.add),
                 (Eoh.b,), (Selk.b,))
            S.op("pool", lambda e, h=h: e.tensor_copy(out=Self_.ap[:, h, :], in_=Eoh.ap[:, 8 + 4 * d + h, :]),
                 (Eoh.b,), (Self_.b,))
            S.op("pool", lambda e, h=h: e.tensor_scalar(out=Selq.ap[:, h, :], in0=Eoh.ap[:, 8 + 4 * d + h, :],
                                                        scalar1=-1.0, scalar2=None, op0=ALU.mult),
                 (Eoh.b,), (Selq.b,))
        S.barrier()
        Dg = A.alloc((8, 5, 128), BF16)
        for g in range(8):
            for j in range(5):
                S.op("dve", lambda e, g=g, j=j: e.tensor_scalar(out=Dg.ap[:, g, j, :], in0=ident_f.ap,
                                                              scalar1=cw.ap[:, j, g:g + 1], scalar2=None,
                                                              op0=ALU.mult), (ident_f.b, cw.b), (Dg.b,))

        NB = 256
        NH = NB + 4
        xblk = [A.alloc((8, NH), BF16) for _ in range(2)]
        lxb = [S.new_lane(f"mx{d}{i}") for i in range(2)]
        Ab = A.alloc((8, NH), BF16)
        YSs = [A.alloc((8, NB), BF16) for _ in range(2)]
        lys = [S.new_lane(f"mys{d}{i}") for i in range(2)]
        lvd = [S.new_lane(f"mvd{d}{i}") for i in range(4)]
        NBLK = T // NB + 1
        graw = A.alloc((NB,), F32, parts=16)
        g1t = A.alloc((NB,), F32, parts=16)
        lft = A.alloc((NB,), F32, parts=16)
        Bt = A.alloc((NB,), F32, parts=16)
        comb = A.alloc((NB,), F32, parts=16)
        Bm = A.alloc((4,), F32, parts=16)
        EK = [A.alloc((NB,), F32) for _ in range(2)]
        qeT = [A.alloc((4, NB), BF16) for _ in range(2)]
        keT = [A.alloc((4, NB), BF16) for _ in range(2)]
        v_tok = [A.alloc((D,), BF16) for _ in range(4)]
        E_end = [A.alloc((4, 4), F32) for _ in range(2)]
        C = make_chain(4, 256, True, f"ml{d}")
        dsm = A.alloc((8,), F32)
        den = A.alloc((4,), F32)
        rden = A.alloc((4,), F32)
        hout = A.alloc((D,), F32)
        lh = [S.new_lane(f"mh{d}{i}") for i in range(2)]
        if d == 0:
            h0s = [A.alloc((D,), BF16) for _ in range(2)]
        else:
            og = [A.alloc((D,), BF16) for _ in range(4)]
            h0t = [A.alloc((D,), BF16) for _ in range(2)]
            hsq = A.alloc((D,), F32)
            gtmp = hsq
            ss4 = A.alloc((4,), F32)
            sd4 = A.alloc((4,), F32)
            rs4 = A.alloc((4,), F32)
            ybt = [A.alloc((D,), BF16) for _ in range(2)]
            lyb = [S.new_lane(f"myb{i}") for i in range(2)]
        yb_cols = yb_d.rearrange("(r w) c -> w r c", w=GW)

        if d == 0:
            blocks = [("x", 0)] + [("c", 256 * i) for i in range(T // NB)]
        else:
            blocks = [("x", 0)] + [("c", 256 * i) for i in reversed(range(T // NB))]
        tiles = [0, 1] if d == 0 else [1, 0]
        cnt = [0]

        def prep(bn, src, p0):
            xb = xblk[bn % 2]
            zl = zr = False
            if src == "x":
                dma("sp", xb.ap[:, :, 2:2 + NB], xnT_x[:, :, 0:NB], lxb[bn % 2], (), (xb.b,))
                zl = zr = True
            elif p0 == 0:
                dma("sp", xb.ap[:, :, 2:NH], xnT_c[:, :, 0:NB + 2], lxb[bn % 2], (), (xb.b,))
                zl = True
            elif p0 == T - NB:
                dma("sp", xb.ap[:, :, 0:NB + 2], xnT_c[:, :, p0 - 2:T], lxb[bn % 2], (), (xb.b,))
                zr = True
            else:
                dma("sp", xb.ap, xnT_c[:, :, p0 - 2:p0 + NB + 2], lxb[bn % 2], (), (xb.b,))
            outf = src == "c"
            qe, ke, Ee = qeT[bn % 2], keT[bn % 2], E_end[bn % 2]
            YS = YSs[bn % 2]
            cslot = bn if d == 0 else (0 if bn == 0 else NBLK - bn)
            for g in range(8 if d == 0 else 0):
                bank = g % 2
                W = Wq if g < 4 else Wk
                gc = (g % 4) * 128

                def am(e, W=W, gc=gc, bank=bank, xb=xb):
                    for k in range(8):
                        i = e.matmul(psf(bank, 128, NH), lhsT=W.ap[:, k, gc:gc + 128], rhs=xb.ap[:, k, :],
                                     start=(k == 0), stop=(k == 7))
                    return i
                S.op("pe", am, (W.b, xb.b), (psbuf[bank],))
                S.op("act", lambda e, g=g, bank=bank: e.activation(out=Ab.ap[:, g, :], in_=psf(bank, 128, NH),
                                                                   func=AF.Identity, bias=bqk.ap[:, g:g + 1]),
                     (psbuf[bank], bqk.b), (Ab.b,))
                if g % 2 == 1:
                    yield
            if zl and d == 0:
                memset("pool", Ab, Ab.ap[:, :, 0:2], 0.0)
            if zr and d == 0:
                memset("pool", Ab, Ab.ap[:, :, NH - 2:NH], 0.0)
            for g in range(8 if d == 0 else 0):
                bank = g % 2

                def cm(e, g=g, bank=bank):
                    for j in range(5):
                        i = e.matmul(psf(bank, 128, NB), lhsT=Dg.ap[:, g, j, :], rhs=Ab.ap[:, g, j:j + NB],
                                     start=(j == 0), stop=(j == 4))
                    return i
                S.op("pe", cm, (Dg.b, Ab.b), (psbuf[bank],))
                S.op("act", lambda e, g=g, bank=bank, YS=YS: e.activation(out=YS.ap[:, g, :], in_=psf(bank, 128, NB),
                                                                   func=AF.Silu, bias=cb.ap[:, g:g + 1]),
                     (psbuf[bank], cb.b), (YS.b,))
                if g % 2 == 1:
                    yield
            if d == 0:
                dma("sp", ys_d[cslot], YS.ap, lys[bn % 2], (YS.b,), ())
            else:
                dma("sp", YS.ap, ys_d[cslot], lys[bn % 2], (), (YS.b,))
            def gm(e, xb=xb):
                for k in range(8):
                    i = e.matmul(psf(0, 16, NB), lhsT=Wgt.ap[:, k, :], rhs=xb.ap[:, k, 2:2 + NB], start=(k == 0),
                                 stop=(k == 7))
                return i
            S.op("pe", gm, (Wgt.b, xb.b), (psbuf[0],))
            S.op("act", lambda e: e.activation(out=graw.ap, in_=psf(0, 16, NB), func=AF.Identity, bias=bgt.ap),
                 (psbuf[0], bgt.b), (graw.b,))
            S.op("act", lambda e: e.activation(out=g1t.ap, in_=graw.ap, func=AF.Exp, scale=-1.0), (graw.b,),
                 (g1t.b,))
            S.op("act", lambda e: e.activation(out=g1t.ap, in_=g1t.ap, func=AF.Ln, bias=1.0), (g1t.b,), (g1t.b,))
            S.op("dve", lambda e: e.tensor_scalar(out=lft.ap, in0=g1t.ap, scalar1=-1.0, scalar2=None, op0=ALU.mult),
                 (g1t.b,), (lft.b,))
            S.op("dve", lambda e: e.tensor_tensor_scan(out=Bt.ap, data0=rmask.ap[0:16, 0:NB], data1=lft.ap,
                                                       initial=0.0, op0=ALU.mult, op1=ALU.add),
                 (rmask.b, lft.b), (Bt.b,))
            S.op("dve", lambda e: e.tensor_copy(out=Bm.ap, in_=Bt.ap[:, 63:NB:64]), (Bt.b,), (Bm.b,))
            if d == 0:
                S.op("dve", lambda e: e.tensor_tensor(
                    out=comb.ap.rearrange("p (c t) -> p c t", t=64),
                    in0=Bt.ap[:, 63:NB:64].unsqueeze(2).broadcast_to([16, NB // 64, 64]),
                    in1=Bt.ap.rearrange("p (c t) -> p c t", t=64), op=ALU.subtract), (Bt.b,), (comb.b,))
            else:
                S.op("dve", lambda e: e.tensor_tensor(out=comb.ap, in0=Bt.ap, in1=lft.ap, op=ALU.subtract),
                     (Bt.b, lft.b), (comb.b,))
            S.op("dve", lambda e: e.tensor_copy(out=comb.ap[0:8, :], in_=graw.ap[0:8, :]), (graw.b,), (comb.b,))
            yield
            for h in range(4):
                ek = EK[0]
                S.op("pe", lambda e, h=h: e.matmul(psf(0, 128, NB), lhsT=Selk.ap[:, h, :], rhs=comb.ap, start=True,
                                                   stop=True), (Selk.b, comb.b), (psbuf[0],))
                S.op("act", lambda e, ek=ek: e.activation(out=ek.ap, in_=psf(0, 128, NB), func=AF.Exp),
                     (psbuf[0],), (ek.b,))
                S.op("dve", lambda e, h=h, ek=ek, ke=ke, YS=YS: e.tensor_tensor(out=ke.ap[:, h, :], in0=YS.ap[:, 4 + h, :],
                                                                       in1=ek.ap, op=ALU.mult), (YS.b, ek.b),
                     (ke.b,))
                S.op("pe", lambda e, h=h: e.matmul(psf(1, 128, 4), lhsT=Self_.ap[:, h, :], rhs=Bm.ap, start=True,
                                                   stop=True), (Self_.b, Bm.b), (psbuf[1],))
                S.op("act", lambda e, h=h, Ee=Ee: e.activation(out=Ee.ap[:, h, :], in_=psf(1, 128, 4), func=AF.Exp),
                     (psbuf[1],), (Ee.b,))
                yield
                if outf:
                    eq = EK[1]
                    S.op("pe", lambda e, h=h: e.matmul(psf(1, 128, NB), lhsT=Selq.ap[:, h, :], rhs=comb.ap,
                                                       start=True, stop=True), (Selq.b, comb.b), (psbuf[1],))
                    S.op("act", lambda e, eq=eq: e.activation(out=eq.ap, in_=psf(1, 128, NB), func=AF.Exp),
                         (psbuf[1],), (eq.b,))
                    S.op("dve", lambda e, h=h, eq=eq, qe=qe, YS=YS: e.scalar_tensor_tensor(
                        out=qe.ap[:, h, :], in0=YS.ap[:, h, :], scalar=float(128 ** -0.5), in1=eq.ap, op0=ALU.mult,
                        op1=ALU.mult), (YS.b, eq.b), (qe.b,))
            yield
            for ti in tiles:
                vt = v_tok[(bn % 2) * 2 + ti]
                if d == 0:
                    for half in range(2):
                        bank = half

                        def vm(e, ti=ti, half=half, bank=bank, xb=xb):
                            for k in range(8):
                                i = e.matmul(psf(bank), lhsT=xb.ap[:, k, 2 + ti * 128:2 + (ti + 1) * 128],
                                             rhs=Wv.ap[:, k, half * 512:(half + 1) * 512], start=(k == 0), stop=(k == 7))
                            return i
                        S.op("pe", vm, (Wv.b, xb.b), (psbuf[bank],))
                        S.op("dve", lambda e, vt=vt, half=half, bank=bank: e.tensor_tensor(
                            out=vt.ap[:, half * 512:(half + 1) * 512], in0=psf(bank),
                            in1=bv_bc.ap[:, half * 512:(half + 1) * 512], op=ALU.add), (psbuf[bank], bv_bc.b), (vt.b,))
                    dma("sp", v_d[cslot, ti], vt.ap, lvd[((bn % 2) * 2 + ti)], (vt.b,), ())
                else:
                    dma("sp", vt.ap, v_d[cslot, ti], lvd[((bn % 2) * 2 + ti)], (), (vt.b,))
                if d == 1 and outf:
                    ogt = og[(bn % 2) * 2 + ti]
                    for half in range(2):
                        bank = half

                        def om_(e, ti=ti, half=half, bank=bank, xb=xb):
                            for k in range(8):
                                i = e.matmul(psf(bank, 64), lhsT=xb.ap[:, k, 2 + ti * 128:2 + ti * 128 + 64],
                                             rhs=Wo.ap[:, k, half * 512:(half + 1) * 512], start=(k == 0),
                                             stop=(k == 7))
                            return i
                        S.op("pe", om_, (Wo.b, xb.b), (psbuf[bank],))
                        S.op("dve", lambda e, half=half, bank=bank: e.tensor_tensor(
                            out=gtmp.ap[0:64, half * 512:(half + 1) * 512], in0=psf(bank, 64),
                            in1=bo_bc.ap[0:64, half * 512:(half + 1) * 512], op=ALU.add), (psbuf[bank], bo_bc.b),
                            (gtmp.b,))
                    S.op("act", lambda e, ogt=ogt: e.activation(out=ogt.ap[0:64, :], in_=gtmp.ap[0:64, :],
                                                                func=AF.Sigmoid), (gtmp.b,), (ogt.b,))
                yield

        def chain(bn, src, p0):
            outf = src == "c"
            qe, ke, Ee = qeT[bn % 2], keT[bn % 2], E_end[bn % 2]
            for ti in tiles:
                vt_n = cnt[0]
                cnt[0] += 1
                vt = v_tok[(bn % 2) * 2 + ti]
                if d == 1 and outf:
                    ogt = og[(bn % 2) * 2 + ti]
                if outf:
                    chunks = [(0, True), (64, False)] if d == 0 else [(64, False), (0, True)]
                else:
                    chunks = [(0, False), (64, False)] if d == 0 else [(64, False), (0, False)]
                ecol = {0: 2 * ti, 64: 2 * ti + 1}
                yield from chain_tile(C, d, qe, ke, slice(ti * 128, (ti + 1) * 128), vt, Ee, ecol, chunks,
                                      64 if outf else 0, with_den=True)
                if outf:
                    w = p0 // 128 + ti
                    S.op("act", lambda e: e.activation(out=dsm.ap[0:64, 0:4], in_=PS[0:64, 3, 192:196], func=AF.Copy),
                         (psbuf[3],), (dsm.b,))
                    S.op("act", lambda e: e.activation(out=dsm.ap[0:64, 4:8], in_=PS[0:64, 3, 320:324], func=AF.Copy),
                         (psbuf[3],), (dsm.b,))
                    S.op("dve", lambda e: e.tensor_tensor(out=den.ap[0:64, :], in0=dsm.ap[0:64, 0:4],
                                                          in1=dsm.ap[0:64, 4:8], op=ALU.add), (dsm.b,), (den.b,))
                    S.op("dve", lambda e: e.tensor_scalar(out=dsm.ap[0:64, 0:4], in0=den.ap[0:64, :], scalar1=-1.0,
                                                          scalar2=None, op0=ALU.mult), (den.b,), (dsm.b,))
                    S.op("dve", lambda e: e.tensor_tensor(out=den.ap[0:64, :], in0=den.ap[0:64, :],
                                                          in1=dsm.ap[0:64, 0:4], op=ALU.max), (den.b, dsm.b), (den.b,))
                    S.op("dve", lambda e: e.tensor_scalar(out=den.ap[0:64, :], in0=den.ap[0:64, :], scalar1=1.0,
                                                          scalar2=None, op0=ALU.max), (den.b,), (den.b,))
                    S.op("dve", lambda e: e.reciprocal(out=rden.ap[0:64, :], in_=den.ap[0:64, :]), (den.b,), (rden.b,))
                    ops = PS[0:64, 6:8, :].rearrange("p b (h v) -> p (b h) v", v=256)
                    S.op("dve", lambda e: e.tensor_tensor(
                        out=hout.ap[0:64, :].rearrange("p (h v) -> p h v", h=4), in0=ops,
                        in1=rden.ap[0:64, :].unsqueeze(2).broadcast_to([64, 4, 256]), op=ALU.mult),
                        (psbuf[6], psbuf[7], rden.b), (hout.b,))
                    if d == 0:
                        ht = h0s[vt_n % 2]
                        S.op("act", lambda e, ht=ht: e.activation(out=ht.ap[0:64, :], in_=hout.ap[0:64, :],
                                                                  func=AF.Copy), (hout.b,), (ht.b,))
                        dma("sp", o0_d[w * 64:(w + 1) * 64, :], ht.ap[0:64, :], lh[vt_n % 2], (ht.b,), ())
                    else:
                        ht = h0t[vt_n % 2]
                        dma("sp", ht.ap[0:64, :], o0_d[w * 64:(w + 1) * 64, :], lh[vt_n % 2], (), (ht.b,))
                        S.op("dve", lambda e, ht=ht: e.tensor_tensor(out=hout.ap[0:64, :], in0=hout.ap[0:64, :],
                                                                    in1=ht.ap[0:64, :], op=ALU.add),
                             (hout.b, ht.b), (hout.b,))
                        S.op("pool", lambda e: e.tensor_tensor(out=hsq.ap[0:64, :], in0=hout.ap[0:64, :],
                                                               in1=hout.ap[0:64, :], op=ALU.mult), (hout.b,),
                             (hsq.b,))
                        S.op("dve", lambda e: e.tensor_reduce(out=ss4.ap[0:64, :],
                                                              in_=hsq.ap[0:64, :].rearrange("p (h v) -> p h v", h=4),
                                                              axis=AX.X, op=ALU.add), (hsq.b,), (ss4.b,))
                        S.op("act", lambda e: e.activation(out=sd4.ap[0:64, :], in_=ss4.ap[0:64, :], func=AF.Sqrt,
                                                           bias=eps_c.ap[0:64, :], scale=1.0 / 256), (ss4.b, eps_c.b),
                             (sd4.b,))
                        S.op("dve", lambda e: e.reciprocal(out=rs4.ap[0:64, :], in_=sd4.ap[0:64, :]), (sd4.b,),
                             (rs4.b,))
                        S.op("dve", lambda e: e.tensor_tensor(
                            out=hout.ap[0:64, :].rearrange("p (h v) -> p h v", h=4),
                            in0=hout.ap[0:64, :].rearrange("p (h v) -> p h v", h=4),
                            in1=rs4.ap[0:64, :].unsqueeze(2).broadcast_to([64, 4, 256]), op=ALU.mult),
                            (hout.b, rs4.b), (hout.b,))
                        S.op("pool", lambda e: e.tensor_tensor(out=hsq.ap[0:64, :], in0=hout.ap[0:64, :],
                                                               in1=mln_bc.ap[0:64, :], op=ALU.mult),
                             (hout.b, mln_bc.b), (hsq.b,))
                        yb = ybt[vt_n % 2]
                        S.op("dve", lambda e, yb=yb, ogt=ogt: e.tensor_tensor(out=yb.ap[0:64, :], in0=hsq.ap[0:64, :],
                                                                             in1=ogt.ap[0:64, :], op=ALU.mult),
                             (hsq.b, ogt.b), (yb.b,))
                        dma("sp", yb_cols[w, 0:64, :], yb.ap[0:64, :], lyb[vt_n % 2], (yb.b,), ())
                yield

        pipeline([(prep(bn, *blk), chain(bn, *blk)) for bn, blk in enumerate(blocks)])
        S.barrier()
        A.reset(m0)

    def phase_merge():
        m0 = A.mark()
        Wpa = A.alloc((8, D), BF16)
        Wpb = A.alloc((8, D), BF16)
        Wo = A.alloc((8, D), BF16)
        Wga = A.alloc((8, D), BF16)
        Wgb = A.alloc((8, D), BF16)
        lws = [S.new_lane(f"gw{i}") for i in range(5)]
        for t, src, ln in ((Wpa, w_pa, lws[0]), (Wpb, w_pb, lws[1]), (Wo, w_out, lws[2])):
            dma("pool", t.ap, src.rearrange("(k p) n -> p k n", p=128), ln, (), (t.b,))
        load_w(Wga, O_GA, D, lws[3])
        load_w(Wgb, O_GB, D, lws[4])
        Wrt = A.alloc((8, 36), F32)
        dma_small(Wrt.ap, w_rt.rearrange("(k p) n -> p k n", p=128), (), (Wrt.b,))
        bga_bc = A.alloc((D,), F32)
        bgb_bc = A.alloc((D,), F32)
        g1_bc = A.alloc((D,), F32)
        G2_bc = A.alloc((D,), F32)
        SH2_bc = A.alloc((D,), F32)
        brt_bc = A.alloc((36,), F32)
        bc_load_in(bga_bc, b_in[0:1, O_GA:O_GA + D], D)
        bc_load_in(bgb_bc, b_in[0:1, O_GB:O_GB + D], D)
        bc_load(g1_bc, 6)
        bc_load(G2_bc, 4)
        bc_load(SH2_bc, 5)
        bc_load_in(brt_bc, b_rt[0:1, :], 36)
        S.barrier()
        yat = [A.alloc((D,), BF16) for _ in range(2)]
        ybt = [A.alloc((D,), BF16) for _ in range(2)]
        xt = [A.alloc((D,), F32) for _ in range(2)]
        xnt = [A.alloc((8, 128), BF16) for _ in range(2)]
        ll = [[S.new_lane(f"gl{j}{i}") for i in range(2)] for j in range(4)]
        yaT = A.alloc((8, 128), BF16)
        ybT = A.alloc((8, 128), BF16)
        yT = A.alloc((8, 128), BF16)
        sga = A.alloc((512,), F32)
        sgb = A.alloc((512,), F32)
        yf = A.alloc((D,), F32)
        t2 = A.alloc((512,), F32)
        ybf2 = [A.alloc((D,), BF16) for _ in range(2)]
        yf2 = A.alloc((D,), F32)
        xm = [A.alloc((D,), F32) for _ in range(2)]
        lxm = [S.new_lane(f"gxm{i}") for i in range(2)]
        junk = A.alloc((D,), BF16)
        ss = A.alloc((1,), F32)
        sd = A.alloc((1,), F32)
        rs = A.alloc((1,), F32)
        h2 = A.alloc((D,), F32)
        h2T2 = [A.alloc((8, 128), F32) for _ in range(2)]
        h2st = [A.alloc((8, 512), BF16) for _ in range(2)]
        lh2 = [S.new_lane(f"gh2{i}") for i in range(2)]
        lg = A.alloc((36,), F32)
        sm = A.alloc((64,), F32)
        elm = A.alloc((32,), F32)
        oh1 = A.alloc((32,), F32)
        oh2 = A.alloc((32,), F32)
        top8 = A.alloc((8,), F32)

        def dv(fn, r, w):
            S.op("dve", fn, r, w)
        def trp(e, src, bank):
            pv = psb(bank).rearrange("p (k t) -> p k t", k=8)
            for k in range(8):
                ins = e.transpose(out=pv[:, k, :], in_=src.ap[:, k * 128:(k + 1) * 128], identity=ident_b.ap)
            return ins

        def mmw(e, lhs, W, half, bank):
            for k in range(8):
                ins = e.matmul(psf(bank), lhsT=lhs.ap[:, k, :], rhs=W.ap[:, k, half * 512:(half + 1) * 512],
                               start=(k == 0), stop=(k == 7))
            return ins

        def sA(i):
            a = i % 2
            ya, yb, x, xn = yat[a], ybt[a], xt[a], xnt[a]
            rows = slice(i * 128, (i + 1) * 128)
            ybf = ybf2[a]
            dma("sp", ya.ap, ya_d[rows, :], ll[0][a], (), (ya.b,))
            dma("sp", yb.ap, yb_d[rows, :], ll[1][a], (), (yb.b,))
            dma("sp", x.ap, xv[rows, :], ll[2][a], (), (x.b,))
            dma("sp", xn.ap, xnT_r[:, :, i * 128:(i + 1) * 128], ll[3][a], (), (xn.b,))
            S.op("pe", lambda e, ya=ya: trp(e, ya, 0), (ya.b, ident_b.b), (psbuf[0],))
            S.op("act", lambda e: e.activation(out=yaT.ap, in_=psb(0).rearrange("p (k t) -> p k t", k=8), func=AF.Copy),
                 (psbuf[0],), (yaT.b,))
            S.op("pe", lambda e, yb=yb: trp(e, yb, 1), (yb.b, ident_b.b), (psbuf[1],))
            S.op("dve", lambda e: e.tensor_copy(out=ybT.ap, in_=psb(1).rearrange("p (k t) -> p k t", k=8)),
                 (psbuf[1],), (ybT.b,))
            for half in range(2):
                hs = slice(half * 512, (half + 1) * 512)
                S.op("pe", lambda e, xn=xn, half=half: mmw(e, xn, Wga, half, 2), (xn.b, Wga.b), (psbuf[2],))
                dv(lambda e, hs=hs: e.tensor_tensor(out=t2.ap, in0=psf(2), in1=bga_bc.ap[:, hs], op=ALU.add),
                   (psbuf[2], bga_bc.b), (t2.b,))
                S.op("act", lambda e: e.activation(out=sga.ap, in_=t2.ap, func=AF.Sigmoid), (t2.b,), (sga.b,))
                S.op("pe", lambda e, half=half: mmw(e, yaT, Wpa, half, 3), (yaT.b, Wpa.b), (psbuf[3],))
                dv(lambda e, hs=hs: e.tensor_tensor(out=yf.ap[:, hs], in0=psf(3), in1=sga.ap, op=ALU.mult),
                   (psbuf[3], sga.b), (yf.b,))
                S.op("pe", lambda e, xn=xn, half=half: mmw(e, xn, Wgb, half, 2), (xn.b, Wgb.b), (psbuf[2],))
                dv(lambda e, hs=hs: e.tensor_tensor(out=t2.ap, in0=psf(2), in1=bgb_bc.ap[:, hs], op=ALU.add),
                   (psbuf[2], bgb_bc.b), (t2.b,))
                S.op("act", lambda e: e.activation(out=sgb.ap, in_=t2.ap, func=AF.Sigmoid), (t2.b,), (sgb.b,))
                S.op("pe", lambda e, half=half: mmw(e, ybT, Wpb, half, 3), (ybT.b, Wpb.b), (psbuf[3],))
                dv(lambda e: e.tensor_tensor(out=t2.ap, in0=psf(3), in1=sgb.ap, op=ALU.mult), (psbuf[3], sgb.b),
                   (t2.b,))
                S.op("pool", lambda e, hs=hs: e.tensor_tensor(out=ybf.ap[:, hs], in0=yf.ap[:, hs], in1=t2.ap,
                                                              op=ALU.add), (yf.b, t2.b), (ybf.b,))

        def sB(i):
            a = i % 2
            x = xt[a]
            rows = slice(i * 128, (i + 1) * 128)
            ybf = ybf2[a]
            h2T = h2T2[a]
            S.op("pe", lambda e, ybf=ybf: trp(e, ybf, 4), (ybf.b, ident_b.b), (psbuf[4],))
            S.op("act", lambda e: e.activation(out=yT.ap, in_=psb(4).rearrange("p (k t) -> p k t", k=8), func=AF.Copy),
                 (psbuf[4],), (yT.b,))
            xmt = xm[a]
            for half in range(2):
                hs = slice(half * 512, (half + 1) * 512)
                S.op("pe", lambda e, half=half: mmw(e, yT, Wo, half, 5), (yT.b, Wo.b), (psbuf[5],))
                dv(lambda e, hs=hs, half=half: e.tensor_tensor(out=yf2.ap[:, hs], in0=psf(5), in1=g1_bc.ap[:, hs],
                                                               op=ALU.mult), (psbuf[5], g1_bc.b), (yf2.b,))
                S.op("pool", lambda e, hs=hs, xmt=xmt, x=x: e.tensor_tensor(out=xmt.ap[:, hs], in0=yf2.ap[:, hs],
                                                                           in1=x.ap[:, hs], op=ALU.add),
                     (yf2.b, x.b), (xmt.b,))
            dma("sp", xmid_d[rows, :], xmt.ap, lxm[a], (xmt.b,), ())
            S.op("act", lambda e, xmt=xmt: e.activation(out=junk.ap, in_=xmt.ap, func=AF.Square, accum_out=ss.ap),
                 (xmt.b,), (junk.b, ss.b))
            rstd_ops(ss, sd, rs, D)
            dv(lambda e, xmt=xmt: e.scalar_tensor_tensor(out=h2.ap, in0=xmt.ap, scalar=rs.ap, in1=G2_bc.ap,
                                                         op0=ALU.mult, op1=ALU.mult), (xmt.b, rs.b, G2_bc.b), (h2.b,))
            S.op("pool", lambda e: e.tensor_tensor(out=h2.ap, in0=h2.ap, in1=SH2_bc.ap, op=ALU.add),
                 (h2.b, SH2_bc.b), (h2.b,))

            def trf(e):
                pv = PS[:, 6:8, :].rearrange("p b (k t) -> p (b k) t", t=128)
                for k in range(8):
                    ins = e.transpose(out=pv[:, k, :], in_=h2.ap[:, k * 128:(k + 1) * 128], identity=ident_f.ap)
                return ins
            S.op("pe", trf, (h2.b, ident_f.b), (psbuf[6], psbuf[7]))
            S.op("act", lambda e: e.activation(out=h2T.ap, in_=PS[:, 6:8, :].rearrange("p b (k t) -> p (b k) t", t=128),
                                               func=AF.Copy), (psbuf[6], psbuf[7]), (h2T.b,))
            st = h2st[(i // 4) % 2]
            S.op("pool", lambda e, st=st, c=i % 4: e.tensor_copy(out=st.ap[:, :, c * 128:(c + 1) * 128], in_=h2T.ap),
                 (h2T.b,), (st.b,))
            if i % 4 == 3:
                dma("sp", h2T_d[:, :, (i - 3) * 128:(i + 1) * 128], st.ap, lh2[(i // 4) % 2], (st.b,), ())


        def sC(i):
            a = i % 2
            h2T = h2T2[a]
            def lgm(e):
                for k in range(8):
                    ins = e.matmul(psf(4, 128, 36), lhsT=h2T.ap[:, k, :], rhs=Wrt.ap[:, k, :], start=(k == 0),
                                   stop=(k == 7))
                return ins
            S.op("pe", lgm, (h2T.b, Wrt.b), (psbuf[4],))
            gl = lg.ap[:, 0:4]
            el = lg.ap[:, 4:36]
            gmax, ngmax, gsum, gw = (sm.ap[:, j:j + 1] for j in range(4))
            goh = sm.ap[:, 4:8]
            gexp = sm.ap[:, 8:12]
            pen = sm.ap[:, 12:16]
            dvv, ed, w1, w2, w1g, w2g = (sm.ap[:, j:j + 1] for j in range(16, 22))
            dv(lambda e: e.tensor_tensor(out=lg.ap, in0=psf(4, 128, 36), in1=brt_bc.ap, op=ALU.add),
               (psbuf[4], brt_bc.b), (lg.b,))
            dv(lambda e: e.tensor_reduce(out=gmax, in_=gl, axis=AX.X, op=ALU.max), (lg.b,), (sm.b,))
            dv(lambda e: e.tensor_scalar(out=goh, in0=gl, scalar1=gmax, scalar2=None, op0=ALU.is_equal),
               (lg.b, sm.b), (sm.b,))
            dv(lambda e: e.tensor_scalar(out=ngmax, in0=gmax, scalar1=-1.0, scalar2=None, op0=ALU.mult), (sm.b,),
               (sm.b,))
            S.op("act", lambda e: e.activation(out=gexp, in_=gl, func=AF.Exp, bias=ngmax, accum_out=gsum),
                 (lg.b, sm.b), (sm.b,))
            dv(lambda e: e.reciprocal(out=gw, in_=gsum), (sm.b,), (sm.b,))
            dv(lambda e: e.tensor_scalar(out=pen, in0=goh, scalar1=-1.0, scalar2=1e30, op0=ALU.add, op1=ALU.mult),
               (sm.b,), (sm.b,))
            dv(lambda e: e.tensor_tensor(out=elm.ap.rearrange("p (g x) -> p g x", g=4),
                                         in0=el.rearrange("p (g x) -> p g x", g=4),
                                         in1=pen.unsqueeze(2).broadcast_to([128, 4, 8]), op=ALU.add),
               (lg.b, sm.b), (elm.b,))
            dv(lambda e: e.max(out=top8.ap, in_=elm.ap), (elm.b,), (top8.b,))
            dv(lambda e: e.tensor_scalar(out=oh1.ap, in0=elm.ap, scalar1=top8.ap[:, 0:1], scalar2=None,
                                         op0=ALU.is_equal), (elm.b, top8.b), (oh1.b,))
            dv(lambda e: e.tensor_scalar(out=oh2.ap, in0=elm.ap, scalar1=top8.ap[:, 1:2], scalar2=None,
                                         op0=ALU.is_equal), (elm.b, top8.b), (oh2.b,))
            dv(lambda e: e.tensor_tensor(out=dvv, in0=top8.ap[:, 1:2], in1=top8.ap[:, 0:1], op=ALU.subtract),
               (top8.b,), (sm.b,))
            S.op("act", lambda e: e.activation(out=ed, in_=dvv, func=AF.Exp), (sm.b,), (sm.b,))
            dv(lambda e: e.tensor_scalar(out=ed, in0=ed, scalar1=1.0, scalar2=None, op0=ALU.add), (sm.b,), (sm.b,))
            dv(lambda e: e.reciprocal(out=w1, in_=ed), (sm.b,), (sm.b,))
            dv(lambda e: e.tensor_scalar(out=w2, in0=w1, scalar1=-1.0, scalar2=1.0, op0=ALU.mult, op1=ALU.add),
               (sm.b,), (sm.b,))
            dv(lambda e: e.tensor_tensor(out=w1g, in0=w1, in1=gw, op=ALU.mult), (sm.b,), (sm.b,))
            dv(lambda e: e.tensor_tensor(out=w2g, in0=w2, in1=gw, op=ALU.mult), (sm.b,), (sm.b,))
            dv(lambda e: e.tensor_scalar(out=oh1.ap, in0=oh1.ap, scalar1=w1g, scalar2=None, op0=ALU.mult),
               (oh1.b, sm.b), (oh1.b,))
            dv(lambda e, i=i: e.scalar_tensor_tensor(out=comb_all.ap[:, i, :], in0=oh2.ap, scalar=w2g, in1=oh1.ap,
                                                     op0=ALU.mult, op1=ALU.add), (oh2.b, oh1.b, sm.b), (comb_all.b,))

        pipeline_k([sA, sB, sC], list(range(TO // 128)))
        if debug:
            dbg_comb = nc.dram_tensor("dbg_comb", [128, 32, 32], F32, kind="ExternalOutput").ap()
            dma_small(dbg_comb, comb_all.ap, (comb_all.b,), ())
        S.barrier()
        A.reset(m0)

    def phase_moe():
        m0 = A.mark()
        HT = TO // 2
        h2h = A.alloc((8, HT), BF16)
        acc = A.alloc((HT // 128, D), F32)
        lh = S.new_lane("moe_h2")
        Wg = [A.alloc((8, DE), BF16) for _ in range(2)]
        Wu = [A.alloc((8, DE), BF16) for _ in range(2)]
        Wd = [A.alloc((4, D), BF16) for _ in range(2)]
        lw = [[S.new_lane(f"moew{j}{i}") for i in range(2)] for j in range(3)]
        sg = [A.alloc((512,), BF16) for _ in range(2)]
        h1T = [A.alloc((4, 512), BF16) for _ in range(2)]
        g2_bc = A.alloc((D,), F32)
        fin_bc = A.alloc((D,), F32)
        bc_load(g2_bc, 7)
        bc_load_in(fin_bc, fing[0:1, :], D)
        S.barrier()
        xmt = [A.alloc((D,), F32) for _ in range(2)]
        lxm = [S.new_lane(f"moex{i}") for i in range(2)]
        ot = [A.alloc((D,), F32) for _ in range(2)]
        lot = [S.new_lane(f"moeo{i}") for i in range(2)]
        tt = A.alloc((D,), F32)
        tts = [tt, tt]
        junk = A.alloc((D,), BF16)
        ssf = [A.alloc((1,), F32) for _ in range(2)]
        sdf = [A.alloc((1,), F32) for _ in range(2)]
        rsf = [A.alloc((1,), F32) for _ in range(2)]
        accb = [Buf(f"acc{i}") for i in range(HT // 128)]
        fcnt = [0]

        def final_tile(hf, tl):
            gt = hf * (HT // 128) + tl
            a = fcnt[0] % 2
            fcnt[0] += 1
            xm = xmt[a]
            o = ot[a]
            t_ = tts[a]
            rows = slice(gt * 128, (gt + 1) * 128)
            dma("sp", xm.ap, xmid_d[rows, :], lxm[a], (), (xm.b,))
            S.op("dve", lambda e: e.tensor_tensor(out=t_.ap, in0=acc.ap[:, tl, :], in1=g2_bc.ap, op=ALU.mult),
                 (accb[tl], g2_bc.b), (t_.b,))
            S.op("pool", lambda e: e.tensor_tensor(out=t_.ap, in0=t_.ap, in1=xm.ap, op=ALU.add),
                 (t_.b, xm.b), (t_.b,))
            S.op("act", lambda e: e.activation(out=junk.ap, in_=t_.ap, func=AF.Square, accum_out=ssf[a].ap), (t_.b,),
                 (junk.b, ssf[a].b))
            rstd_ops(ssf[a], sdf[a], rsf[a], D)
            S.op("dve", lambda e: e.scalar_tensor_tensor(out=o.ap, in0=t_.ap, scalar=rsf[a].ap, in1=fin_bc.ap,
                                                         op0=ALU.mult, op1=ALU.mult),
                 (t_.b, rsf[a].b, fin_bc.b), (o.b,))
            out_dmas.append(dma("sp", out[rows, :], o.ap, lot[a], (o.b,), ()))

        out_dmas = []
        nw = 0
        for hf in range(2):
            dma("sp", h2h.ap, h2T_d[:, :, hf * HT:(hf + 1) * HT], lh, (), (h2h.b,))
            for ex in range(NE):
                a = nw % 2
                nw += 1
                wg, wu, wd = Wg[a], Wu[a], Wd[a]
                dma("pool", wg.ap, w_gate[ex].rearrange("(k p) n -> p k n", p=128), lw[0][a], (), (wg.b,))
                dma("pool", wu.ap, w_up[ex].rearrange("(k p) n -> p k n", p=128), lw[1][a], (), (wu.b,))
                dma("pool", wd.ap, w_down[ex].rearrange("(k p) n -> p k n", p=128), lw[2][a], (), (wd.b,))
                for blk in range(HT // 512):
                    h1 = h1T[blk % 2]
                    ts = slice(blk * 512, (blk + 1) * 512)
                    for jc in range(4):
                        bg, bu = (0, 2) if jc % 2 == 0 else (1, 3)
                        sgt = sg[jc % 2]

                        def gmm(e, W, bank, jc=jc, ts=ts):
                            for k in range(8):
                                ins = e.matmul(psf(bank), lhsT=W.ap[:, k, jc * 128:(jc + 1) * 128], rhs=h2h.ap[:, k, ts],
                                               start=(k == 0), stop=(k == 7))
                            return ins
                        S.op("pe", lambda e, wg=wg, bg=bg, f=gmm: f(e, wg, bg), (wg.b, h2h.b), (psbuf[bg],))
                        S.op("act", lambda e, sgt=sgt, bg=bg: e.activation(out=sgt.ap, in_=psf(bg), func=AF.Silu),
                             (psbuf[bg],), (sgt.b,))
                        S.op("pe", lambda e, wu=wu, bu=bu, f=gmm: f(e, wu, bu), (wu.b, h2h.b), (psbuf[bu],))
                        S.op("dve", lambda e, sgt=sgt, bu=bu, h1=h1, jc=jc: e.tensor_tensor(
                            out=h1.ap[:, jc, :], in0=psf(bu), in1=sgt.ap, op=ALU.mult), (psbuf[bu], sgt.b), (h1.b,))
                    for t4 in range(4):
                        tl = blk * 4 + t4
                        gt = hf * (HT // 128) + tl
                        for dh in range(2):
                            bank = 4 + (t4 * 2 + dh) % 4

                            def dmm(e, bank=bank, t4=t4, dh=dh, h1=h1, wd=wd):
                                for jc in range(4):
                                    ins = e.matmul(psf(bank), lhsT=h1.ap[:, jc, t4 * 128:(t4 + 1) * 128],
                                                   rhs=wd.ap[:, jc, dh * 512:(dh + 1) * 512], start=(jc == 0),
                                                   stop=(jc == 3))
                                return ins
                            S.op("pe", dmm, (h1.b, wd.b), (psbuf[bank],))
                            accv = acc.ap[:, tl, dh * 512:(dh + 1) * 512]
                            cs = comb_all.ap[:, gt, ex:ex + 1]
                            if ex == 0:
                                S.op("dve", lambda e, accv=accv, cs=cs, bank=bank: e.tensor_scalar(
                                    out=accv, in0=psf(bank), scalar1=cs, scalar2=None, op0=ALU.mult),
                                    (psbuf[bank], comb_all.b), (accb[tl],))
                            else:
                                S.op("dve", lambda e, accv=accv, cs=cs, bank=bank: e.scalar_tensor_tensor(
                                    out=accv, in0=psf(bank), scalar=cs, in1=accv, op0=ALU.mult, op1=ALU.add),
                                    (psbuf[bank], comb_all.b, accb[tl]), (accb[tl],))
                        if ex == NE - 1:
                            final_tile(hf, tl)
        A.reset(m0)
        return out_dmas

    order = ["mod", "norm", "hgrn", "mlstm", "merge", "moe"]
    upto = order.index(stop_after) if stop_after is not None else len(order) - 1
    phase_mod()
    A.reset(base_mark)
    finals = []
    if upto >= 1:
        phase_norm()
        A.reset(base_mark)
    if upto >= 2:
        phase_hgrn(0)
        phase_hgrn(1)
    if upto >= 3:
        phase_mlstm(0)
        phase_mlstm(1)
    if upto >= 4:
        phase_merge()
    if upto >= 5:
        finals = phase_moe()
    else:
        S.barrier()
        fin = A.alloc((D,), F32)
        memset("pool", fin, fin.ap, 0.0)
        finals = [dma("sp", out[0:128, :], fin.ap, L_setup, (fin.b,), ())]
    lastper = {}
    for o_ in finals:
        lastper[id(o_.lane)] = o_
    with nc.Block() as block:
        S.emit(block, eng_sems, final_waits=list(lastper.values()))
    return nc


def _core_inputs(inp, core):
    b = core // 2
    flip = core % 2 == 1
    f32 = np.float32
    x = np.asarray(inp["x"][b], f32)
    ctx = np.asarray(inp["ctx"][b], f32)
    w_in = np.asarray(inp["w_in"][0], f32)
    b_in = np.asarray(inp["b_in"][0], f32)
    lbl = np.asarray(inp["hg_lb_logits"], f32)
    convw = np.asarray(inp["ml_conv_w"][0], f32)
    if flip:
        x = x[::-1]
        ctx = ctx[::-1]
        perm = np.arange(DIN)
        perm[O_HF0:O_HF0 + D] = np.arange(O_HF1, O_HF1 + D)
        perm[O_HF1:O_HF1 + D] = np.arange(O_HF0, O_HF0 + D)
        perm[O_MI:O_MI + 4] = np.arange(O_MI + 4, O_MI + 8)
        perm[O_MI + 4:O_MI + 8] = np.arange(O_MI, O_MI + 4)
        perm[O_MF:O_MF + 4] = np.arange(O_MF + 4, O_MF + 8)
        perm[O_MF + 4:O_MF + 8] = np.arange(O_MF, O_MF + 4)
        w_in = w_in[:, perm]
        b_in = b_in[perm]
        lbl = lbl[::-1]
        convw = convw[::-1]
    cv = np.concatenate([np.asarray(inp["c"][b], f32).reshape(8, 128).T,
                         np.asarray(inp["c_ctx"], f32).reshape(8, 128).T], axis=1)
    m = {
        "xv": np.ascontiguousarray(x), "ctxv": np.ascontiguousarray(ctx), "cvec": np.ascontiguousarray(cv),
        "w_ada": np.asarray(inp["w_ada"][0], f32), "b_ada": np.asarray(inp["b_ada"], f32).reshape(1, -1),
        "n1g": np.asarray(inp["norm1_g"], f32).reshape(1, -1), "n2g": np.asarray(inp["norm2_g"], f32).reshape(1, -1),
        "fing": np.asarray(inp["final_norm_g"], f32).reshape(1, -1),
        "w_in": np.ascontiguousarray(w_in), "b_in": np.ascontiguousarray(b_in).reshape(1, -1),
        "lbl": np.ascontiguousarray(lbl.reshape(4, D)),
        "hgn": np.asarray(inp["hg_norm_g"], f32).reshape(1, -1), "mln": np.asarray(inp["ml_norm_g"], f32).reshape(1, -1),
        "convw": np.ascontiguousarray(convw), "convb": np.asarray(inp["ml_conv_b"], f32).reshape(1, -1),
        "w_pa": np.asarray(inp["w_branch_a"][0], f32), "w_pb": np.asarray(inp["w_branch_b"][0], f32),
        "w_out": np.asarray(inp["w_out"][0], f32),
        "w_rt": np.ascontiguousarray(np.concatenate([np.asarray(inp["w_group"][0], f32),
                                                     np.asarray(inp["w_router"][0], f32)], axis=1)),
        "b_rt": np.concatenate([np.asarray(inp["b_group"][0], f32), np.asarray(inp["b_router"][0], f32)]).reshape(1, -1),
        "w_gate": np.asarray(inp["w_gate"][0], f32), "w_up": np.asarray(inp["w_up"][0], f32),
        "w_down": np.asarray(inp["w_down"][0], f32),
    }
    return m


_NC_CACHE = {}


def kernel(**inputs):
    if "nc" not in _NC_CACHE:
        _NC_CACHE["nc"] = build_program()
    nc = _NC_CACHE["nc"]
    in_maps = [_core_inputs(inputs, c) for c in range(8)]
    res = run_bass_kernel_spmd(nc, in_maps, core_ids=list(range(8)))
    outp = np.empty((4, T, D), np.float32)
    for c in range(8):
        o = np.asarray(res.results[c]["out"], np.float32)
        b = c // 2
        if c % 2 == 0:
            outp[b, 0:TO] = o
        else:
            outp[b, TO:T] = o[::-1]
    return outp
```

```python
import numpy as np
import concourse.bass as bass
import concourse.mybir as mybir
from concourse.bass_utils import run_bass_kernel_spmd

F32 = mybir.dt.float32
BF16 = mybir.dt.bfloat16
U8 = mybir.dt.uint8
AF = mybir.ActivationFunctionType
ALU = mybir.AluOpType
AX = mybir.AxisListType

D = 1024
import os as _os
T = int(_os.environ.get('KDBG_T', 8192))
TO = T // 2
GW = T // 128
CTX = 256
EPS = 1e-6
NE = 32
DE = 512
DIN = 10256
O_HQ, O_HF0, O_HF1, O_HI, O_HG = 0, 1024, 2048, 3072, 4096
O_MQ, O_MK, O_MV, O_MI, O_MF, O_MO, O_GA, O_GB = 5120, 5632, 6144, 7168, 7176, 7184, 8208, 9232


class Lane:
    def __init__(self, sem):
        self.sem = sem
        self.count = 0
        self.last = None


class Buf:
    __slots__ = ("name", "last_w", "readers")

    def __init__(self, name=""):
        self.name = name
        self.last_w = None
        self.readers = []


class TB:
    __slots__ = ("ap", "b")

    def __init__(self, ap, b=None):
        self.ap = ap
        self.b = b if b is not None else Buf()


class Op:
    __slots__ = ("eng", "fn", "deps", "lane", "lane_val", "needs_inc", "inc_idx", "is_dma")

    def __init__(self, eng, fn, lane):
        self.eng = eng
        self.fn = fn
        self.deps = set()
        self.lane = lane
        self.lane_val = 0
        self.needs_inc = False
        self.inc_idx = 0
        self.is_dma = lane is not None


class Sched:
    ENGS = ("pe", "act", "dve", "pool", "sp")

    def __init__(self, nc):
        self.nc = nc
        self.streams = {e: [] for e in self.ENGS}
        self.last_compute = {e: None for e in self.ENGS}
        self.lanes = []
        self.pending = {e: None for e in self.ENGS}

    def new_lane(self, name):
        ln = Lane(self.nc.alloc_semaphore(name=name))
        self.lanes.append(ln)
        return ln

    def barrier(self):
        deps = set()
        for e in self.ENGS:
            if self.last_compute[e] is not None:
                deps.add(self.last_compute[e])
        for ln in self.lanes:
            if ln.last is not None:
                deps.add(ln.last)
        for e in self.ENGS:
            prev = self.pending[e]
            self.pending[e] = set(deps) | (prev if prev else set())

    def op(self, eng, fn, reads=(), writes=(), lane=None):
        o = Op(eng, fn, lane)
        deps = set()
        for b in reads:
            if b.last_w is not None:
                deps.add(b.last_w)
        for b in writes:
            if b.last_w is not None:
                deps.add(b.last_w)
            deps.update(b.readers)
        if self.pending[eng]:
            deps.update(self.pending[eng])
            self.pending[eng] = None
        for b in reads:
            b.readers.append(o)
        for b in writes:
            b.last_w = o
            b.readers = []
        deps.discard(o)
        for d in deps:
            if (not o.is_dma) and (not d.is_dma) and o.eng == "pe" and d.eng == "pe":
                continue
            o.deps.add(d)
            if not d.is_dma:
                d.needs_inc = True
        if lane is not None:
            lane.count += 1
            o.lane_val = 16 * lane.count
            lane.last = o
        else:
            self.last_compute[eng] = o
        self.streams[eng].append(o)
        return o

    def emit(self, block, eng_sems, final_waits=()):
        nc = self.nc
        for e in self.ENGS:
            c = 0
            for o in self.streams[e]:
                if o.needs_inc and not o.is_dma:
                    c += 1
                    o.inc_idx = c
        deco = {"pe": block.tensor, "act": block.scalar, "dve": block.vector, "pool": block.gpsimd,
                "sp": block.sync}

        def make(e):
            def body(eng):
                waited = {}
                for o in self.streams[e]:
                    need = {}
                    for d in o.deps:
                        if d.is_dma:
                            s, v = d.lane.sem, d.lane_val
                        else:
                            s, v = eng_sems[d.eng], d.inc_idx
                        k = id(s)
                        if v > waited.get(k, 0) and v > need.get(k, (None, 0))[1]:
                            need[k] = (s, v)
                    for k, (s, v) in need.items():
                        eng.wait_ge(s, v)
                        waited[k] = v
                    ins = o.fn(eng)
                    if o.is_dma:
                        ins.then_inc(o.lane.sem, 16)
                    elif o.needs_inc:
                        ins.then_inc(eng_sems[e], 1)
                if e == "sp":
                    for d in final_waits:
                        eng.wait_ge(d.lane.sem, d.lane_val)

            return body

        for e in self.ENGS:
            deco[e](make(e))


def _dsz(dt):
    return 4 if dt == F32 else (2 if dt == BF16 else 1)


class Arena:
    def __init__(self, nc, nbytes):
        self.t = nc.alloc_sbuf_tensor("arena", [128, nbytes], U8)
        self.cap = nbytes
        self.off = 0

    def mark(self):
        return self.off

    def reset(self, m):
        self.off = m

    def alloc(self, free, dtype, parts=128):
        free = tuple(int(f) for f in free)
        n = int(np.prod(free)) * _dsz(dtype)
        assert self.off + n <= self.cap, f"SBUF arena overflow {self.off}+{n}>{self.cap}"
        v = self.t[0:parts, self.off:self.off + n].bitcast(dtype)
        self.off += (n + 63) // 64 * 64
        if len(free) == 2:
            v = v.rearrange("p (a b) -> p a b", a=free[0])
        elif len(free) == 3:
            v = v.rearrange("p (a b c) -> p a b c", a=free[0], b=free[1])
        return TB(v)


def build_program(stop_after=None, debug=False):
    nc = bass.Bass("TRN2", target_bir_lowering=False)
    S = Sched(nc)

    def din(name, shape):
        return nc.dram_tensor(name, list(shape), F32, kind="ExternalInput").ap()

    def dscr(name, shape, dt):
        kind = "ExternalOutput" if debug else "Internal"
        return nc.dram_tensor(name, list(shape), dt, kind=kind).ap()

    xv = din("xv", (T, D))
    ctxv = din("ctxv", (CTX, D))
    cvec = din("cvec", (128, 16))
    w_ada = din("w_ada", (D, 6 * D))
    b_ada = din("b_ada", (1, 6 * D))
    n1g = din("n1g", (1, D))
    n2g = din("n2g", (1, D))
    fing = din("fing", (1, D))
    w_in = din("w_in", (D, DIN))
    b_in = din("b_in", (1, DIN))
    lbl = din("lbl", (4, D))
    hgn = din("hgn", (1, D))
    mln = din("mln", (1, D))
    convw = din("convw", (5, D))
    convb = din("convb", (1, D))
    w_pa = din("w_pa", (D, D))
    w_pb = din("w_pb", (D, D))
    w_out = din("w_out", (D, D))
    w_rt = din("w_rt", (D, 36))
    b_rt = din("b_rt", (1, 36))
    NEd = NE if stop_after in (None, 'moe') else 1
    w_gate = din("w_gate", (NEd, D, DE))
    w_up = din("w_up", (NEd, D, DE))
    w_down = din("w_down", (NEd, DE, D))
    out = nc.dram_tensor("out", [TO, D], F32, kind="ExternalOutput").ap()

    xnT_r = dscr("xnT_r", (128, 8, T), BF16)
    xnT_c = dscr("xnT_c", (128, 8, T), BF16)
    xnT_x = dscr("xnT_x", (128, 8, CTX), BF16)
    scr_rows = dscr("scr_rows", (1, 8 * D), F32)
    o0_d = dscr("o0_d", (TO, D), BF16)
    ya_d = dscr("ya_d", (TO, D), BF16)
    yb_d = dscr("yb_d", (TO, D), BF16)
    xmid_d = dscr("xmid_d", (TO, D), F32)
    h2T_d = dscr("h2T_d", (128, 8, TO), BF16)
    ys_d = nc.dram_tensor("ys_d", [T // 256 + 1, 128, 8, 256], BF16, kind="Internal").ap()
    v_d = nc.dram_tensor("v_d", [T // 256 + 1, 2, 128, D], BF16, kind="Internal").ap()

    A = Arena(nc, 196000)
    PS = nc.alloc_psum_tensor("ps", [128, 8, 512], F32)

    def psf(bank, parts=128, n=512):
        return PS[0:parts, bank, 0:n]

    def psb(bank, parts=128):
        return PS[0:parts, bank, :].bitcast(BF16)

    psbuf = [Buf(f"psum{i}") for i in range(8)]

    eng_sems = {e: nc.alloc_semaphore(name=f"sem_{e}") for e in Sched.ENGS}
    L_setup = S.new_lane("setup")

    def dma(eng, out_ap, in_ap, lane, reads=(), writes=()):
        return S.op(eng, lambda e, o=out_ap, i=in_ap: e.dma_start(out=o, in_=i), reads, writes, lane=lane)

    def dma_small(out_ap, in_ap, reads=(), writes=()):
        def f(e, o=out_ap, i=in_ap):
            with nc.allow_non_contiguous_dma(reason="small setup vectors"):
                return e.dma_start(out=o, in_=i)
        return S.op("sp", f, reads, writes, lane=L_setup)

    ident_b = A.alloc((128,), BF16)
    ident_f = A.alloc((128,), F32)
    mask0 = A.alloc((128,), BF16)
    mask1 = A.alloc((128,), BF16)
    rmask = A.alloc((256,), F32)
    ones_b = A.alloc((8,), BF16)
    comb_all = A.alloc((32, 32), F32)
    eps_c = A.alloc((1,), F32)

    def memset(eng, t, ap, val):
        return S.op(eng, lambda e, a=ap, v=val: e.memset(a, v), (), (t.b,))

    def aff(t, ap_out, ap_in, pattern, cmp, fill, base, cm):
        return S.op("pool", lambda e: e.affine_select(out=ap_out, in_=ap_in, pattern=pattern, compare_op=cmp,
                                                      fill=fill, base=base, channel_multiplier=cm),
                    (t.b,), (t.b,))

    memset("pool", ident_b, ident_b.ap, 0.0)
    aff(ident_b, ident_b.ap, ident_b.ap, [[-1, 128]], ALU.not_equal, 1.0, 0, 1)
    memset("pool", ident_f, ident_f.ap, 0.0)
    aff(ident_f, ident_f.ap, ident_f.ap, [[-1, 128]], ALU.not_equal, 1.0, 0, 1)
    memset("pool", mask0, mask0.ap, 1.0)
    aff(mask0, mask0.ap, mask0.ap, [[1, 128]], ALU.is_ge, 0.0, 0, -1)
    memset("pool", mask0, mask0.ap[0:64, 64:128], 0.0)
    memset("pool", mask1, mask1.ap, 1.0)
    aff(mask1, mask1.ap, mask1.ap, [[-1, 128]], ALU.is_ge, 0.0, 0, 1)
    memset("pool", mask1, mask1.ap[64:128, 0:64], 0.0)
    memset("pool", rmask, rmask.ap, 1.0)
    memset("pool", rmask, rmask.ap[:, 0:256:64], 0.0)
    memset("pool", ones_b, ones_b.ap, 1.0)
    memset("pool", eps_c, eps_c.ap, EPS)
    MASKS = (mask0, mask1)

    base_mark = A.mark()

    def rstd_ops(ss, sd, rs, n, parts=128):
        S.op("act", lambda e: e.activation(out=sd.ap, in_=ss.ap, func=AF.Sqrt, bias=eps_c.ap[0:parts, :],
                                           scale=1.0 / n), (ss.b, eps_c.b), (sd.b,))
        S.op("dve", lambda e: e.reciprocal(out=rs.ap, in_=sd.ap), (sd.b,), (rs.b,))

    def phase_mod():
        c_sb = A.alloc((16,), F32)
        sil = A.alloc((16,), F32)
        bada = A.alloc((6 * D,), F32, parts=1)
        n1row = A.alloc((D,), F32, parts=1)
        n2row = A.alloc((D,), F32, parts=1)
        modr = A.alloc((6 * D,), F32, parts=1)
        modx = A.alloc((2 * D,), F32, parts=1)
        wblk = [A.alloc((8, 512), F32) for _ in range(2)]
        lw = [S.new_lane(f"wada{i}") for i in range(2)]
        dma_small(c_sb.ap, cvec, (), (c_sb.b,))
        dma_small(bada.ap, b_ada, (), (bada.b,))
        dma_small(n1row.ap, n1g, (), (n1row.b,))
        dma_small(n2row.ap, n2g, (), (n2row.b,))
        S.barrier()
        S.op("act", lambda e: e.activation(out=sil.ap, in_=c_sb.ap, func=AF.Silu), (c_sb.b,), (sil.b,))
        for j in range(12):
            wb = wblk[j % 2]
            dma("sp", wb.ap, w_ada[:, j * 512:(j + 1) * 512].rearrange("(k p) n -> p k n", p=128), lw[j % 2],
                (), (wb.b,))
            ba, bb = (0, 1) if j % 2 == 0 else (2, 3)

            def mmf(e, wb=wb, bank=ba, col=0):
                for k in range(8):
                    i = e.matmul(psf(bank, 1), lhsT=sil.ap[:, col + k:col + k + 1], rhs=wb.ap[:, k, :],
                                 start=(k == 0), stop=(k == 7))
                return i
            S.op("pe", mmf, (sil.b, wb.b), (psbuf[ba],))
            S.op("dve", lambda e, bank=ba, j=j: e.tensor_tensor(out=modr.ap[:, j * 512:(j + 1) * 512],
                                                                in0=psf(bank, 1),
                                                                in1=bada.ap[:, j * 512:(j + 1) * 512], op=ALU.add),
                 (psbuf[ba], bada.b), (modr.b,))
            if j < 4:
                S.op("pe", lambda e, wb=wb, bank=bb: mmf(e, wb, bank, 8), (sil.b, wb.b), (psbuf[bb],))
                S.op("dve", lambda e, bank=bb, j=j: e.tensor_tensor(out=modx.ap[:, j * 512:(j + 1) * 512],
                                                                    in0=psf(bank, 1),
                                                                    in1=bada.ap[:, j * 512:(j + 1) * 512],
                                                                    op=ALU.add),
                     (psbuf[bb], bada.b), (modx.b,))
        mr = A.alloc((8 * D,), F32, parts=1)

        def stt(out_ap, in0, scalar, in1, op0, op1, r, w):
            S.op("dve", lambda e: e.scalar_tensor_tensor(out=out_ap, in0=in0, scalar=scalar, in1=in1, op0=op0,
                                                         op1=op1), r, w)
        stt(mr.ap[:, 0:D], modr.ap[:, D:2 * D], 1.0, n1row.ap, ALU.add, ALU.mult, (modr.b, n1row.b), (mr.b,))
        S.op("dve", lambda e: e.tensor_copy(out=mr.ap[:, D:2 * D], in_=modr.ap[:, 0:D]), (modr.b,), (mr.b,))
        stt(mr.ap[:, 2 * D:3 * D], modx.ap[:, D:2 * D], 1.0, n1row.ap, ALU.add, ALU.mult, (modx.b, n1row.b), (mr.b,))
        S.op("dve", lambda e: e.tensor_copy(out=mr.ap[:, 3 * D:4 * D], in_=modx.ap[:, 0:D]), (modx.b,), (mr.b,))
        stt(mr.ap[:, 4 * D:5 * D], modr.ap[:, 4 * D:5 * D], 1.0, n2row.ap, ALU.add, ALU.mult, (modr.b, n2row.b),
            (mr.b,))
        S.op("dve", lambda e: e.tensor_copy(out=mr.ap[:, 5 * D:6 * D], in_=modr.ap[:, 3 * D:4 * D]), (modr.b,),
             (mr.b,))
        S.op("dve", lambda e: e.tensor_copy(out=mr.ap[:, 6 * D:7 * D], in_=modr.ap[:, 2 * D:3 * D]), (modr.b,),
             (mr.b,))
        S.op("dve", lambda e: e.tensor_copy(out=mr.ap[:, 7 * D:8 * D], in_=modr.ap[:, 5 * D:6 * D]), (modr.b,),
             (mr.b,))
        dma_small(scr_rows, mr.ap, (mr.b,), ())
        S.barrier()

    def bc_load(t, row_idx):
        src = scr_rows[0:1, row_idx * D:(row_idx + 1) * D].partition_broadcast(128)
        dma_small(t.ap.unsqueeze(1), src, (), (t.b,))

    def bc_load_in(t, dram_row_ap, n):
        src = dram_row_ap.partition_broadcast(128)
        dma_small(t.ap.unsqueeze(1), src, (), (t.b,))

    def pipeline_k(stages, items):
        K = len(stages)
        for t in range(len(items) + K - 1):
            for st_i in range(K):
                i = t - st_i
                if 0 <= i < len(items):
                    stages[st_i](items[i])

    def phase_norm():
        G = A.alloc((D,), F32)
        SH = A.alloc((D,), F32)
        Gx = A.alloc((D,), F32)
        SHx = A.alloc((D,), F32)
        bc_load(G, 0)
        bc_load(SH, 1)
        bc_load(Gx, 2)
        bc_load(SHx, 3)
        S.barrier()
        NBUF = 4
        xt = [A.alloc((D,), F32) for _ in range(NBUF)]
        lx = [S.new_lane(f"nx{i}") for i in range(NBUF)]
        junk = A.alloc((D,), BF16)
        ss = [A.alloc((1,), F32) for _ in range(NBUF)]
        sd = [A.alloc((1,), F32) for _ in range(NBUF)]
        rs = [A.alloc((1,), F32) for _ in range(NBUF)]
        tmp = [A.alloc((D,), F32) for _ in range(NBUF)]
        hb = [A.alloc((D,), BF16) for _ in range(NBUF)]
        stage = [A.alloc((8, 512), BF16) for _ in range(2)]
        lst = [S.new_lane(f"nst{i}") for i in range(2)]
        xcol = xv.rearrange("(r w) d -> w r d", w=GW)

        jobs = []
        for i in range(T // 128):
            jobs.append((xv[i * 128:(i + 1) * 128, :], G, SH, xnT_r, i * 128))
        for w in range(GW):
            jobs.append((xcol[w], G, SH, xnT_c, w * 128))
        for i in range(CTX // 128):
            jobs.append((ctxv[i * 128:(i + 1) * 128, :], Gx, SHx, xnT_x, i * 128))
        stt = {"nst": 0, "st": None, "lane": None, "off": 0, "cnt": 0}

        def front(n):
            src, g, sh, dst, off = jobs[n]
            a = n % NBUF
            x, s1, s2, r1, t1, h1 = xt[a], ss[a], sd[a], rs[a], tmp[a], hb[a]
            dma("sp", x.ap, src, lx[a], (), (x.b,))
            S.op("act", lambda e, x=x, s1=s1: e.activation(out=junk.ap, in_=x.ap, func=AF.Square,
                                                           accum_out=s1.ap), (x.b,), (junk.b, s1.b))
            rstd_ops(s1, s2, r1, D)
            S.op("dve", lambda e, x=x, r1=r1, g=g, t1=t1: e.scalar_tensor_tensor(
                out=t1.ap, in0=x.ap, scalar=r1.ap, in1=g.ap, op0=ALU.mult, op1=ALU.mult),
                (x.b, r1.b, g.b), (t1.b,))
            S.op("pool", lambda e, t1=t1, sh=sh, h1=h1: e.tensor_tensor(out=h1.ap, in0=t1.ap, in1=sh.ap,
                                                                         op=ALU.add), (t1.b, sh.b), (h1.b,))
            bank = n % 2

            def tr(e, h1=h1, bank=bank):
                pv = psb(bank).rearrange("p (k t) -> p k t", k=8)
                for k in range(8):
                    i = e.transpose(out=pv[:, k, :], in_=h1.ap[:, k * 128:(k + 1) * 128], identity=ident_b.ap)
                return i
            S.op("pe", tr, (h1.b, ident_b.b), (psbuf[bank],))

        def back(n):
            src, g, sh, dst, off = jobs[n]
            bank = n % 2
            first = (n == 0) or (jobs[n - 1][3] is not dst) or (off % 512 == 0)
            if first:
                stt["st"] = stage[stt["nst"] % 2]
                stt["lane"] = lst[stt["nst"] % 2]
                stt["nst"] += 1
                stt["off"] = off
                stt["cnt"] = 0
            st = stt["st"]
            S.op("act", lambda e, st=st, c=stt["cnt"], bank=bank: e.activation(
                out=st.ap[:, :, c * 128:(c + 1) * 128], in_=psb(bank).rearrange("p (k t) -> p k t", k=8),
                func=AF.Copy), (psbuf[bank],), (st.b,))
            stt["cnt"] += 1
            last = (n == len(jobs) - 1) or (jobs[n + 1][3] is not dst) or ((off + 128) % 512 == 0)
            if last:
                dma("sp", dst[:, :, stt["off"]:stt["off"] + stt["cnt"] * 128], st.ap[:, :, 0:stt["cnt"] * 128],
                    stt["lane"], (st.b,), ())
        pipeline_k([front, back], list(range(len(jobs))))
        S.barrier()

    def pipeline(stages):
        prev = None
        for p, c in stages:
            if prev is None:
                for _ in p:
                    pass
            else:
                ga, gb = p, prev
                da = db = False
                while not (da and db):
                    if not da:
                        try:
                            next(ga)
                        except StopIteration:
                            da = True
                    if not db:
                        try:
                            next(gb)
                        except StopIteration:
                            db = True
            prev = c
        if prev is not None:
            for _ in prev:
                pass

    class ChainCfg:
        pass

    def make_chain(H, DV, with_den, name):
        C = ChainCfg()
        C.H, C.DV = H, DV
        C.S2 = [A.alloc((H, DV), F32) for _ in range(2)]
        C.cur = 0
        C.Spb = [A.alloc((H, DV), BF16) for _ in range(2)]
        C.ke_tok = [A.alloc((H, 128), BF16) for _ in range(2)]
        C.at_sb = [A.alloc((H, 128), BF16) for _ in range(2)]
        if with_den:
            C.n = A.alloc((H,), F32)
            C.np_ = A.alloc((H,), F32)
            C.npb = [A.alloc((H,), BF16) for _ in range(2)]
        memset("pool", C.S2[0], C.S2[0].ap, 0.0)
        if with_den:
            memset("pool", C.n, C.n.ap, 0.0)
        C.cnt = 0
        return C

    def chain_tile(C, d, qeT, keT, tsl, v_tok, E_end, ecol, chunks, R, with_den=False):
        H, DV = C.H, C.DV
        n = C.cnt
        C.cnt += 1
        ke_tok = C.ke_tok[n % 2]
        at_sb = C.at_sb[n % 2]
        hpb = 512 // DV

        def tr(e):
            pv = psb(2).rearrange("p (h k) -> p h k", h=8)
            for h in range(H):
                i = e.transpose(out=pv[:, h, :], in_=keT.ap[:, h, tsl], identity=ident_b.ap)
            return i
        S.op("pe", tr, (keT.b, ident_b.b), (psbuf[2],))
        S.op("act", lambda e: e.activation(out=ke_tok.ap, in_=psb(2).rearrange("p (h k) -> p h k", h=8)[:, 0:H, :],
                                           func=AF.Copy), (psbuf[2],), (ke_tok.b,))
        t0 = tsl.start
        if R > 0:
            if H <= 4:
                def at(e):
                    pv = psf(3).rearrange("p (h t) -> p h t", h=4)
                    for h in range(H):
                        i = e.matmul(pv[0:R, h, 0:R], lhsT=keT.ap[:, h, t0:t0 + R],
                                     rhs=qeT.ap[:, h, t0:t0 + R], start=True, stop=True)
                    return i
                S.op("pe", at, (keT.b, qeT.b), (psbuf[3],))
                S.op("dve", lambda e: e.tensor_tensor(
                    out=at_sb.ap[0:R, :, 0:R], in0=psf(3).rearrange("p (h t) -> p h t", h=4)[0:R, 0:H, 0:R],
                    in1=MASKS[d].ap[0:R, 0:R].unsqueeze(1).broadcast_to([R, H, R]), op=ALU.mult),
                    (psbuf[3], MASKS[d].b), (at_sb.b,))
            else:
                for half in range(2):
                    def at2(e, half=half):
                        pv = psf(3).rearrange("p (h t) -> p h t", h=4)
                        for hh in range(4):
                            h = half * 4 + hh
                            i = e.matmul(pv[0:R, hh, 0:R], lhsT=keT.ap[:, h, t0:t0 + R],
                                         rhs=qeT.ap[:, h, t0:t0 + R], start=True, stop=True)
                        return i
                    S.op("pe", at2, (keT.b, qeT.b), (psbuf[3],))
                    S.op("dve", lambda e, half=half: e.tensor_tensor(
                        out=at_sb.ap[0:R, half * 4:half * 4 + 4, 0:R],
                        in0=psf(3).rearrange("p (h t) -> p h t", h=4)[0:R, :, 0:R],
                        in1=MASKS[d].ap[0:R, 0:R].unsqueeze(1).broadcast_to([R, 4, R]), op=ALU.mult),
                        (psbuf[3], MASKS[d].b), (at_sb.b,))
        yield
        spb_used = {}
        for ci, (lo, outf) in enumerate(chunks):
            ec = ecol[lo]
            Spb = C.Spb[ci % 2]
            Sc = C.S2[C.cur]
            Sn = C.S2[1 - C.cur]
            C.cur = 1 - C.cur
            if outf:
                S.op("pool", lambda e, ec=ec, Sc=Sc, Spb=Spb: e.tensor_tensor(
                    out=Spb.ap, in0=Sc.ap, in1=E_end.ap[:, :, ec:ec + 1].broadcast_to([128, H, DV]), op=ALU.mult),
                    (Sc.b, E_end.b), (Spb.b,))
                spb_used[lo] = Spb

            def kv(e, lo=lo):
                for h in range(H):
                    bank = 4 + h // hpb
                    col = (h % hpb) * DV
                    i = e.matmul(PS[:, bank, col:col + DV], lhsT=ke_tok.ap[lo:lo + 64, h, :],
                                 rhs=v_tok.ap[lo:lo + 64, h * DV:(h + 1) * DV], start=True, stop=True)
                return i
            S.op("pe", kv, (ke_tok.b, v_tok.b), (psbuf[4], psbuf[5]))

            def su(e, ec=ec, Sc=Sc, Sn=Sn):
                for h in range(H):
                    bank = 4 + h // hpb
                    col = (h % hpb) * DV
                    i = e.scalar_tensor_tensor(out=Sn.ap[:, h, :], in0=Sc.ap[:, h, :], scalar=E_end.ap[:, h, ec:ec + 1],
                                               in1=PS[:, bank, col:col + DV], op0=ALU.mult, op1=ALU.add)
                return i
            S.op("dve", su, (Sc.b, E_end.b, psbuf[4], psbuf[5]), (Sn.b,))
            if with_den:
                npb = C.npb[ci % 2]
                S.op("dve", lambda e, ec=ec: e.tensor_tensor(out=C.np_.ap, in0=C.n.ap, in1=E_end.ap[:, :, ec],
                                                            op=ALU.mult), (C.n.b, E_end.b), (C.np_.b,))
                if outf:
                    S.op("act", lambda e, npb=npb: e.activation(out=npb.ap, in_=C.np_.ap, func=AF.Copy),
                         (C.np_.b,), (npb.b,))
                    spb_used[("n", lo)] = npb

                def kn(e, lo=lo):
                    for h in range(H):
                        i = e.matmul(PS[:, 3, 64 + h:65 + h], lhsT=ke_tok.ap[lo:lo + 64, h, :],
                                     rhs=ones_b.ap[lo:lo + 64, 0:1], start=True, stop=True)
                    return i
                S.op("pe", kn, (ke_tok.b, ones_b.b), (psbuf[3],))
                S.op("dve", lambda e: e.tensor_tensor(out=C.n.ap, in0=C.np_.ap, in1=PS[:, 3, 64:64 + H],
                                                      op=ALU.add), (C.np_.b, psbuf[3]), (C.n.b,))
            yield
        if R > 0:
            outs = [lo for (lo, outf) in chunks if outf]
            rd = [at_sb.b, v_tok.b, qeT.b] + [spb_used[lo].b for lo in outs]

            def om(e):
                for h in range(H):
                    bank = 6 + h // hpb
                    col = (h % hpb) * DV
                    e.matmul(PS[0:R, bank, col:col + DV], lhsT=at_sb.ap[0:R, h, 0:R],
                             rhs=v_tok.ap[0:R, h * DV:(h + 1) * DV], start=True, stop=False)
                    for j, lo in enumerate(outs):
                        i = e.matmul(PS[lo:lo + 64, bank, col:col + DV], lhsT=qeT.ap[:, h, t0 + lo:t0 + lo + 64],
                                     rhs=spb_used[lo].ap[:, h, :], start=False, stop=True)
                return i
            S.op("pe", om, rd, (psbuf[6], psbuf[7]))
            if with_den:
                rd2 = [at_sb.b, ones_b.b, qeT.b] + [spb_used[("n", lo)].b for lo in outs]

                def dn(e):
                    for h in range(H):
                        e.matmul(PS[0:R, 3, 192 + h:193 + h], lhsT=at_sb.ap[0:R, h, 0:R], rhs=ones_b.ap[0:R, 0:1],
                                 start=True, stop=True)
                        for lo in outs:
                            i = e.matmul(PS[lo:lo + 64, 3, 320 + h:321 + h], lhsT=qeT.ap[:, h, t0 + lo:t0 + lo + 64],
                                         rhs=spb_used[("n", lo)].ap[:, h:h + 1], start=True, stop=True)
                    return i
                S.op("pe", dn, rd2, (psbuf[3],))
        yield

    def load_w(t, col0, ncols, lane, eng="pool"):
        return dma(eng, t.ap, w_in[:, col0:col0 + ncols].rearrange("(k p) n -> p k n", p=128), lane, (), (t.b,))

    def phase_hgrn(d):
        m0 = A.mark()
        Wq = A.alloc((8, D), BF16)
        Wf = A.alloc((8, D), BF16)
        Wi = A.alloc((8, D), BF16)
        lws = [S.new_lane(f"hw{d}{i}") for i in range(4)]
        load_w(Wq, O_HQ, D, lws[0])
        load_w(Wf, O_HF0 if d == 0 else O_HF1, D, lws[1])
        load_w(Wi, O_HI, D, lws[2])
        if d == 1:
            Wg = A.alloc((8, D), BF16)
            load_w(Wg, O_HG, D, lws[3])
            bg_bc = A.alloc((D,), F32)
            bc_load_in(bg_bc, b_in[0:1, O_HG:O_HG + D], D)
            hgn_bc = A.alloc((D,), F32)
            bc_load_in(hgn_bc, hgn[0:1, :], D)
        bi_bc = A.alloc((D,), F32)
        bc_load_in(bi_bc, b_in[0:1, O_HI:O_HI + D], D)
        bq = A.alloc((8,), F32)
        bf = A.alloc((8,), F32)
        l0 = A.alloc((8,), F32)
        l1 = A.alloc((8,), F32)
        lb = A.alloc((8,), F32)
        oml = A.alloc((8,), F32)
        noml = A.alloc((8,), F32)
        fo = O_HF0 if d == 0 else O_HF1
        dma_small(bq.ap, b_in[0, O_HQ:O_HQ + D].rearrange("(h p) -> p h", p=128), (), (bq.b,))
        dma_small(bf.ap, b_in[0, fo:fo + D].rearrange("(h p) -> p h", p=128), (), (bf.b,))
        dma_small(l0.ap, lbl[2 * d, :].rearrange("(h p) -> p h", p=128), (), (l0.b,))
        dma_small(l1.ap, lbl[2 * d + 1, :].rearrange("(h p) -> p h", p=128), (), (l1.b,))
        S.barrier()
        S.op("dve", lambda e: e.tensor_tensor(out=l0.ap, in0=l0.ap, in1=l1.ap, op=ALU.subtract), (l0.b, l1.b),
             (l0.b,))
        S.op("act", lambda e: e.activation(out=lb.ap, in_=l0.ap, func=AF.Sigmoid), (l0.b,), (lb.b,))
        S.op("dve", lambda e: e.tensor_scalar(out=oml.ap, in0=lb.ap, scalar1=-1.0, scalar2=1.0, op0=ALU.mult,
                                              op1=ALU.add), (lb.b,), (oml.b,))
        S.op("dve", lambda e: e.tensor_scalar(out=noml.ap, in0=oml.ap, scalar1=-1.0, scalar2=None, op0=ALU.mult),
             (oml.b,), (noml.b,))

        NB = 256
        xblk = [A.alloc((8, NB), BF16) for _ in range(2)]
        lxb = [S.new_lane(f"hx{d}{i}") for i in range(2)]
        X1 = A.alloc((8, NB), F32)
        X2 = A.alloc((8, NB), F32)
        X3 = A.alloc((8, NB), F32)
        QS = A.alloc((8, NB), BF16)
        qeT = [A.alloc((8, NB), BF16) for _ in range(2)]
        keT = [A.alloc((8, NB), BF16) for _ in range(2)]
        v_tok = [A.alloc((D,), BF16) for _ in range(4)]
        E_end = [A.alloc((8, 4), F32) for _ in range(2)]
        C = make_chain(8, 128, False, f"hg{d}")
        if d == 0:
            o0s = [A.alloc((D,), BF16) for _ in range(2)]
            lo0 = [S.new_lane(f"ho0{i}") for i in range(2)]
        else:
            gate = [A.alloc((D,), BF16) for _ in range(4)]
            o0t = [A.alloc((D,), BF16) for _ in range(2)]
            lo0 = [S.new_lane(f"ho1{i}") for i in range(2)]
            osum = A.alloc((D,), F32)
            osq = A.alloc((D,), F32)
            gtmp = osq
            ss8 = A.alloc((8,), F32)
            sd8 = A.alloc((8,), F32)
            rs8 = A.alloc((8,), F32)
            yat = [A.alloc((D,), BF16) for _ in range(2)]
            lya = [S.new_lane(f"hya{i}") for i in range(2)]

        if d == 0:
            blocks = [("x", 0, False)] + [("r", 256 * i, True) for i in range(TO // NB)]
        else:
            blocks = [("x", 0, False)] + [("r", TO + 256 * i, False) for i in reversed(range(TO // NB))] + \
                     [("r", 256 * i, True) for i in reversed(range(TO // NB))]
        tiles = [0, 1] if d == 0 else [1, 0]
        cnt = [0]

        def prep(bn, src, tok0, outf):
            xb = xblk[bn % 2]
            srcap = xnT_x[:, :, 0:NB] if src == "x" else xnT_r[:, :, tok0:tok0 + NB]
            dma("sp", xb.ap, srcap, lxb[bn % 2], (), (xb.b,))
            qe, ke, Ee = qeT[bn % 2], keT[bn % 2], E_end[bn % 2]
            for h in range(8):
                bank = h % 2

                def fm(e, h=h, bank=bank, xb=xb):
                    for k in range(8):
                        i = e.matmul(psf(bank, 128, NB), lhsT=Wf.ap[:, k, h * 128:(h + 1) * 128], rhs=xb.ap[:, k, :],
                                     start=(k == 0), stop=(k == 7))
                    return i
                S.op("pe", fm, (Wf.b, xb.b), (psbuf[bank],))
                S.op("act", lambda e, h=h, bank=bank: e.activation(out=X1.ap[:, h, :], in_=psf(bank, 128, NB),
                                                                   func=AF.Sigmoid, bias=bf.ap[:, h:h + 1]),
                     (psbuf[bank], bf.b), (X1.b,))
                yield
            for h in range(8):
                S.op("act", lambda e, h=h: e.activation(out=X2.ap[:, h, :], in_=X1.ap[:, h, :], func=AF.Ln,
                                                        bias=lb.ap[:, h:h + 1], scale=oml.ap[:, h:h + 1]),
                     (X1.b, lb.b, oml.b), (X2.b,))
            yield
            for h in range(8):
                S.op("dve", lambda e, h=h: e.tensor_scalar(out=X1.ap[:, h, :], in0=X1.ap[:, h, :],
                                                           scalar1=noml.ap[:, h:h + 1], scalar2=oml.ap[:, h:h + 1],
                                                           op0=ALU.mult, op1=ALU.add), (X1.b, noml.b, oml.b),
                     (X1.b,))
            yield
            for h in range(8):
                S.op("dve", lambda e, h=h: e.tensor_tensor_scan(out=X3.ap[:, h, :], data0=rmask.ap[:, 0:NB],
                                                                data1=X2.ap[:, h, :], initial=0.0, op0=ALU.mult,
                                                                op1=ALU.add), (rmask.b, X2.b), (X3.b,))
            yield
            S.op("act", lambda e, Ee=Ee: e.activation(out=Ee.ap, in_=X3.ap[:, :, 63:NB:64], func=AF.Exp),
                 (X3.b,), (Ee.b,))
            if d == 0:
                S.op("dve", lambda e: e.tensor_tensor(
                    out=X2.ap.rearrange("p h (c t) -> p h c t", t=64),
                    in0=X3.ap[:, :, 63:NB:64].unsqueeze(3).broadcast_to([128, 8, NB // 64, 64]),
                    in1=X3.ap.rearrange("p h (c t) -> p h c t", t=64), op=ALU.subtract), (X3.b,), (X2.b,))
            else:
                S.op("pool", lambda e: e.tensor_tensor(out=X2.ap, in0=X3.ap, in1=X2.ap, op=ALU.subtract),
                     (X3.b, X2.b), (X2.b,))
            S.op("act", lambda e: e.activation(out=X3.ap, in_=X2.ap, func=AF.Exp), (X2.b,), (X3.b,))
            S.op("pool", lambda e, ke=ke: e.tensor_tensor(out=ke.ap, in0=X1.ap, in1=X3.ap, op=ALU.mult),
                 (X1.b, X3.b), (ke.b,))
            yield
            if outf:
                S.op("act", lambda e: e.activation(out=X2.ap, in_=X2.ap, func=AF.Exp, scale=-1.0), (X2.b,),
                     (X2.b,))
                for h in range(8):
                    bank = h % 2

                    def qm(e, h=h, bank=bank, xb=xb):
                        for k in range(8):
                            i = e.matmul(psf(bank, 128, NB), lhsT=Wq.ap[:, k, h * 128:(h + 1) * 128],
                                         rhs=xb.ap[:, k, :], start=(k == 0), stop=(k == 7))
                        return i
                    S.op("pe", qm, (Wq.b, xb.b), (psbuf[bank],))
                    S.op("act", lambda e, h=h, bank=bank: e.activation(out=QS.ap[:, h, :], in_=psf(bank, 128, NB),
                                                                       func=AF.Silu, bias=bq.ap[:, h:h + 1]),
                         (psbuf[bank], bq.b), (QS.b,))
                    yield
                S.op("dve", lambda e, qe=qe: e.tensor_tensor(out=qe.ap, in0=QS.ap, in1=X2.ap, op=ALU.mult),
                     (QS.b, X2.b), (qe.b,))
                yield
            for ti in tiles:
                vt = v_tok[(bn % 2) * 2 + ti]
                for half in range(2):
                    bank = half

                    def vm(e, ti=ti, half=half, bank=bank, xb=xb):
                        for k in range(8):
                            i = e.matmul(psf(bank), lhsT=xb.ap[:, k, ti * 128:(ti + 1) * 128],
                                         rhs=Wi.ap[:, k, half * 512:(half + 1) * 512], start=(k == 0), stop=(k == 7))
                        return i
                    S.op("pe", vm, (Wi.b, xb.b), (psbuf[bank],))
                    S.op("dve", lambda e, vt=vt, half=half, bank=bank: e.tensor_tensor(
                        out=vt.ap[:, half * 512:(half + 1) * 512], in0=psf(bank),
                        in1=bi_bc.ap[:, half * 512:(half + 1) * 512], op=ALU.add), (psbuf[bank], bi_bc.b), (vt.b,))
                yield
                if d == 1 and outf:
                    gt = gate[(bn % 2) * 2 + ti]
                    for half in range(2):
                        bank = half

                        def gm(e, ti=ti, half=half, bank=bank, xb=xb):
                            for k in range(8):
                                i = e.matmul(psf(bank), lhsT=xb.ap[:, k, ti * 128:(ti + 1) * 128],
                                             rhs=Wg.ap[:, k, half * 512:(half + 1) * 512], start=(k == 0),
                                             stop=(k == 7))
                            return i
                        S.op("pe", gm, (Wg.b, xb.b), (psbuf[bank],))
                        S.op("dve", lambda e, half=half, bank=bank: e.tensor_tensor(
                            out=gtmp.ap[:, half * 512:(half + 1) * 512], in0=psf(bank),
                            in1=bg_bc.ap[:, half * 512:(half + 1) * 512], op=ALU.add), (psbuf[bank], bg_bc.b),
                            (gtmp.b,))
                    S.op("act", lambda e, gt=gt: e.activation(out=gt.ap, in_=gtmp.ap, func=AF.Silu), (gtmp.b,),
                         (gt.b,))
                    yield

        def chain(bn, src, tok0, outf):
            qe, ke, Ee = qeT[bn % 2], keT[bn % 2], E_end[bn % 2]
            for ti in tiles:
                vt = v_tok[(bn % 2) * 2 + ti]
                chunks = [(0, outf), (64, outf)] if d == 0 else [(64, outf), (0, outf)]
                ecol = {0: 2 * ti, 64: 2 * ti + 1}
                yield from chain_tile(C, d, qe, ke, slice(ti * 128, (ti + 1) * 128), vt, Ee, ecol, chunks,
                                      128 if outf else 0)
                if outf:
                    vt_n = cnt[0]
                    cnt[0] += 1
                    gtile = (tok0 + ti * 128) // 128
                    ops = PS[:, 6:8, :].rearrange("p b n -> p (b n)")
                    if d == 0:
                        ot = o0s[vt_n % 2]
                        S.op("act", lambda e, ot=ot: e.activation(out=ot.ap, in_=ops, func=AF.Copy),
                             (psbuf[6], psbuf[7]), (ot.b,))
                        dma("sp", o0_d[gtile * 128:(gtile + 1) * 128, :], ot.ap, lo0[vt_n % 2], (ot.b,), ())
                    else:
                        gt = gate[(bn % 2) * 2 + ti]
                        ot = o0t[vt_n % 2]
                        dma("sp", ot.ap, o0_d[gtile * 128:(gtile + 1) * 128, :], lo0[vt_n % 2], (), (ot.b,))
                        S.op("dve", lambda e, ot=ot: e.tensor_tensor(out=osum.ap, in0=ops, in1=ot.ap, op=ALU.add),
                             (psbuf[6], psbuf[7], ot.b), (osum.b,))
                        S.op("pool", lambda e: e.tensor_tensor(out=osq.ap, in0=osum.ap, in1=osum.ap, op=ALU.mult),
                             (osum.b,), (osq.b,))
                        S.op("dve", lambda e: e.tensor_reduce(out=ss8.ap, in_=osq.ap.rearrange("p (h v) -> p h v", h=8),
                                                              axis=AX.X, op=ALU.add), (osq.b,), (ss8.b,))
                        rstd_ops(ss8, sd8, rs8, 128)
                        yield
                        S.op("dve", lambda e: e.tensor_tensor(
                            out=osum.ap.rearrange("p (h v) -> p h v", h=8),
                            in0=osum.ap.rearrange("p (h v) -> p h v", h=8),
                            in1=rs8.ap.unsqueeze(2).broadcast_to([128, 8, 128]), op=ALU.mult), (osum.b, rs8.b),
                            (osum.b,))
                        S.op("pool", lambda e: e.tensor_tensor(out=osq.ap, in0=osum.ap, in1=hgn_bc.ap, op=ALU.mult),
                             (osum.b, hgn_bc.b), (osq.b,))
                        ya = yat[vt_n % 2]
                        S.op("dve", lambda e, ya=ya, gt=gt: e.tensor_tensor(out=ya.ap, in0=osq.ap, in1=gt.ap,
                                                                           op=ALU.mult), (osq.b, gt.b), (ya.b,))
                        dma("sp", ya_d[gtile * 128:(gtile + 1) * 128, :], ya.ap, lya[vt_n % 2], (ya.b,), ())
                    yield

        pipeline([(prep(bn, *blk), chain(bn, *blk)) for bn, blk in enumerate(blocks)])
        S.barrier()
        A.reset(m0)

    def phase_mlstm(d):
        m0 = A.mark()
        Wq = A.alloc((8, 512), BF16)
        Wk = A.alloc((8, 512), BF16)
        Wv = A.alloc((8, D), BF16)
        Wgt = A.alloc((8, 16), BF16)
        lws = [S.new_lane(f"mw{d}{i}") for i in range(5)]
        load_w(Wq, O_MQ, 512, lws[0])
        load_w(Wk, O_MK, 512, lws[1])
        load_w(Wv, O_MV, D, lws[2])
        load_w(Wgt, O_MI, 16, lws[3])
        if d == 1:
            Wo = A.alloc((8, D), BF16)
            load_w(Wo, O_MO, D, lws[4])
            bo_bc = A.alloc((D,), F32)
            bc_load_in(bo_bc, b_in[0:1, O_MO:O_MO + D], D)
            mln_bc = A.alloc((D,), F32)
            bc_load_in(mln_bc, mln[0:1, :], D)
        bv_bc = A.alloc((D,), F32)
        bc_load_in(bv_bc, b_in[0:1, O_MV:O_MV + D], D)
        bqk = A.alloc((8,), F32)
        cw = A.alloc((5, 8), F32)
        cb = A.alloc((8,), F32)
        bgt = A.alloc((1,), F32, parts=16)
        dma_small(bqk.ap, b_in[0, O_MQ:O_MQ + D].rearrange("(g p) -> p g", p=128), (), (bqk.b,))
        for j in range(5):
            dma_small(cw.ap[:, j, :], convw[j, :].rearrange("(g p) -> p g", p=128), (), (cw.b,))
        dma_small(cb.ap, convb[0, :].rearrange("(g p) -> p g", p=128), (), (cb.b,))
        dma_small(bgt.ap, b_in[0, O_MI:O_MI + 16].rearrange("(p o) -> p o", o=1), (), (bgt.b,))
        Eoh = A.alloc((16, 128), F32, parts=16)
        Selk = A.alloc((4, 128), F32, parts=16)
        Selq = A.alloc((4, 128), F32, parts=16)
        Self_ = A.alloc((4, 128), F32, parts=16)
        memset("pool", Eoh, Eoh.ap, 1.0)
        for J in range(16):
            aff(Eoh, Eoh.ap[:, J, :], Eoh.ap[:, J, :], [[0, 128]], ALU.is_equal, 0.0, -J, 1)
        for h in range(4):
            S.op("pool", lambda e, h=h: e.tensor_tensor(out=Selk.ap[:, h, :], in0=Eoh.ap[:, 4 * d + h, :],
                                                        in1=Eoh.ap[:, 8 + 4 * d + h, :], op=ALU.add),
                 (Eoh.b,), (Selk.b,))
            S.op("pool", lambda e, h=h: e.tensor_copy(out=Self_.ap[:, h, :], in_=Eoh.ap[:, 8 + 4 * d + h, :]),
                 (Eoh.b,), (Self_.b,))
            S.op("pool", lambda e, h=h: e.tensor_scalar(out=Selq.ap[:, h, :], in0=Eoh.ap[:, 8 + 4 * d + h, :],
                                                        scalar1=-1.0, scalar2=None, op0=ALU.mult),
                 (Eoh.b,), (Selq.b,))
        S.barrier()
        Dg = A.alloc((8, 5, 128), BF16)
        for g in range(8):
            for j in range(5):
                S.op("dve", lambda e, g=g, j=j: e.tensor_scalar(out=Dg.ap[:, g, j, :], in0=ident_f.ap,
                                                              scalar1=cw.ap[:, j, g:g + 1], scalar2=None,
                                                              op0=ALU.mult), (ident_f.b, cw.b), (Dg.b,))

        NB = 256
        NH = NB + 4
        xblk = [A.alloc((8, NH), BF16) for _ in range(2)]
        lxb = [S.new_lane(f"mx{d}{i}") for i in range(2)]
        Ab = A.alloc((8, NH), BF16)
        YSs = [A.alloc((8, NB), BF16) for _ in range(2)]
        lys = [S.new_lane(f"mys{d}{i}") for i in range(2)]
        lvd = [S.new_lane(f"mvd{d}{i}") for i in range(4)]
        NBLK = T // NB + 1
        graw = A.alloc((NB,), F32, parts=16)
        g1t = A.alloc((NB,), F32, parts=16)
        lft = A.alloc((NB,), F32, parts=16)
        Bt = A.alloc((NB,), F32, parts=16)
        comb = A.alloc((NB,), F32, parts=16)
        Bm = A.alloc((4,), F32, parts=16)
        EK = [A.alloc((NB,), F32) for _ in range(2)]
        qeT = [A.alloc((4, NB), BF16) for _ in range(2)]
        keT = [A.alloc((4, NB), BF16) for _ in range(2)]
        v_tok = [A.alloc((D,), BF16) for _ in range(4)]
        E_end = [A.alloc((4, 4), F32) for _ in range(2)]
        C = make_chain(4, 256, True, f"ml{d}")
        dsm = A.alloc((8,), F32)
        den = A.alloc((4,), F32)
        rden = A.alloc((4,), F32)
        hout = A.alloc((D,), F32)
        lh = [S.new_lane(f"mh{d}{i}") for i in range(2)]
        if d == 0:
            h0s = [A.alloc((D,), BF16) for _ in range(2)]
        else:
            og = [A.alloc((D,), BF16) for _ in range(4)]
            h0t = [A.alloc((D,), BF16) for _ in range(2)]
            hsq = A.alloc((D,), F32)
            gtmp = hsq
            ss4 = A.alloc((4,), F32)
            sd4 = A.alloc((4,), F32)
            rs4 = A.alloc((4,), F32)
            ybt = [A.alloc((D,), BF16) for _ in range(2)]
            lyb = [S.new_lane(f"myb{i}") for i in range(2)]
        yb_cols = yb_d.rearrange("(r w) c -> w r c", w=GW)

        if d == 0:
            blocks = [("x", 0)] + [("c", 256 * i) for i in range(T // NB)]
        else:
            blocks = [("x", 0)] + [("c", 256 * i) for i in reversed(range(T // NB))]
        tiles = [0, 1] if d == 0 else [1, 0]
        cnt = [0]

        def prep(bn, src, p0):
            xb = xblk[bn % 2]
            zl = zr = False
            if src == "x":
                dma("sp", xb.ap[:, :, 2:2 + NB], xnT_x[:, :, 0:NB], lxb[bn % 2], (), (xb.b,))
                zl = zr = True
            elif p0 == 0:
                dma("sp", xb.ap[:, :, 2:NH], xnT_c[:, :, 0:NB + 2], lxb[bn % 2], (), (xb.b,))
                zl = True
            elif p0 == T - NB:
                dma("sp", xb.ap[:, :, 0:NB + 2], xnT_c[:, :, p0 - 2:T], lxb[bn % 2], (), (xb.b,))
                zr = True
            else:
                dma("sp", xb.ap, xnT_c[:, :, p0 - 2:p0 + NB + 2], lxb[bn % 2], (), (xb.b,))
            outf = src == "c"
            qe, ke, Ee = qeT[bn % 2], keT[bn % 2], E_end[bn % 2]
            YS = YSs[bn % 2]
            cslot = bn if d == 0 else (0 if bn == 0 else NBLK - bn)
            for g in range(8 if d == 0 else 0):
                bank = g % 2
                W = Wq if g < 4 else Wk
                gc = (g % 4) * 128

                def am(e, W=W, gc=gc, bank=bank, xb=xb):
                    for k in range(8):
                        i = e.matmul(psf(bank, 128, NH), lhsT=W.ap[:, k, gc:gc + 128], rhs=xb.ap[:, k, :],
                                     start=(k == 0), stop=(k == 7))
                    return i
                S.op("pe", am, (W.b, xb.b), (psbuf[bank],))
                S.op("act", lambda e, g=g, bank=bank: e.activation(out=Ab.ap[:, g, :], in_=psf(bank, 128, NH),
                                                                   func=AF.Identity, bias=bqk.ap[:, g:g + 1]),
                     (psbuf[bank], bqk.b), (Ab.b,))
                if g % 2 == 1:
                    yield
            if zl and d == 0:
                memset("pool", Ab, Ab.ap[:, :, 0:2], 0.0)
            if zr and d == 0:
                memset("pool", Ab, Ab.ap[:, :, NH - 2:NH], 0.0)
            for g in range(8 if d == 0 else 0):
                bank = g % 2

                def cm(e, g=g, bank=bank):
                    for j in range(5):
                        i = e.matmul(psf(bank, 128, NB), lhsT=Dg.ap[:, g, j, :], rhs=Ab.ap[:, g, j:j + NB],
                                     start=(j == 0), stop=(j == 4))
                    return i
                S.op("pe", cm, (Dg.b, Ab.b), (psbuf[bank],))
                S.op("act", lambda e, g=g, bank=bank, YS=YS: e.activation(out=YS.ap[:, g, :], in_=psf(bank, 128, NB),
                                                                   func=AF.Silu, bias=cb.ap[:, g:g + 1]),
                     (psbuf[bank], cb.b), (YS.b,))
                if g % 2 == 1:
                    yield
            if d == 0:
                dma("sp", ys_d[cslot], YS.ap, lys[bn % 2], (YS.b,), ())
            else:
                dma("sp", YS.ap, ys_d[cslot], lys[bn % 2], (), (YS.b,))
            def gm(e, xb=xb):
                for k in range(8):
                    i = e.matmul(psf(0, 16, NB), lhsT=Wgt.ap[:, k, :], rhs=xb.ap[:, k, 2:2 + NB], start=(k == 0),
                                 stop=(k == 7))
                return i
            S.op("pe", gm, (Wgt.b, xb.b), (psbuf[0],))
            S.op("act", lambda e: e.activation(out=graw.ap, in_=psf(0, 16, NB), func=AF.Identity, bias=bgt.ap),
                 (psbuf[0], bgt.b), (graw.b,))
            S.op("act", lambda e: e.activation(out=g1t.ap, in_=graw.ap, func=AF.Exp, scale=-1.0), (graw.b,),
                 (g1t.b,))
            S.op("act", lambda e: e.activation(out=g1t.ap, in_=g1t.ap, func=AF.Ln, bias=1.0), (g1t.b,), (g1t.b,))
            S.op("dve", lambda e: e.tensor_scalar(out=lft.ap, in0=g1t.ap, scalar1=-1.0, scalar2=None, op0=ALU.mult),
                 (g1t.b,), (lft.b,))
            S.op("dve", lambda e: e.tensor_tensor_scan(out=Bt.ap, data0=rmask.ap[0:16, 0:NB], data1=lft.ap,
                                                       initial=0.0, op0=ALU.mult, op1=ALU.add),
                 (rmask.b, lft.b), (Bt.b,))
            S.op("dve", lambda e: e.tensor_copy(out=Bm.ap, in_=Bt.ap[:, 63:NB:64]), (Bt.b,), (Bm.b,))
            if d == 0:
                S.op("dve", lambda e: e.tensor_tensor(
                    out=comb.ap.rearrange("p (c t) -> p c t", t=64),
                    in0=Bt.ap[:, 63:NB:64].unsqueeze(2).broadcast_to([16, NB // 64, 64]),
                    in1=Bt.ap.rearrange("p (c t) -> p c t", t=64), op=ALU.subtract), (Bt.b,), (comb.b,))
            else:
                S.op("dve", lambda e: e.tensor_tensor(out=comb.ap, in0=Bt.ap, in1=lft.ap, op=ALU.subtract),
                     (Bt.b, lft.b), (comb.b,))
            S.op("dve", lambda e: e.tensor_copy(out=comb.ap[0:8, :], in_=graw.ap[0:8, :]), (graw.b,), (comb.b,))
            yield
            for h in range(4):
                ek = EK[0]
                S.op("pe", lambda e, h=h: e.matmul(psf(0, 128, NB), lhsT=Selk.ap[:, h, :], rhs=comb.ap, start=True,
                                                   stop=True), (Selk.b, comb.b), (psbuf[0],))
                S.op("act", lambda e, ek=ek: e.activation(out=ek.ap, in_=psf(0, 128, NB), func=AF.Exp),
                     (psbuf[0],), (ek.b,))
                S.op("dve", lambda e, h=h, ek=ek, ke=ke, YS=YS: e.tensor_tensor(out=ke.ap[:, h, :], in0=YS.ap[:, 4 + h, :],
                                                                       in1=ek.ap, op=ALU.mult), (YS.b, ek.b),
                     (ke.b,))
                S.op("pe", lambda e, h=h: e.matmul(psf(1, 128, 4), lhsT=Self_.ap[:, h, :], rhs=Bm.ap, start=True,
                                                   stop=True), (Self_.b, Bm.b), (psbuf[1],))
                S.op("act", lambda e, h=h, Ee=Ee: e.activation(out=Ee.ap[:, h, :], in_=psf(1, 128, 4), func=AF.Exp),
                     (psbuf[1],), (Ee.b,))
                yield
                if outf:
                    eq = EK[1]
                    S.op("pe", lambda e, h=h: e.matmul(psf(1, 128, NB), lhsT=Selq.ap[:, h, :], rhs=comb.ap,
                                                       start=True, stop=True), (Selq.b, comb.b), (psbuf[1],))
                    S.op("act", lambda e, eq=eq: e.activation(out=eq.ap, in_=psf(1, 128, NB), func=AF.Exp),
                         (psbuf[1],), (eq.b,))
                    S.op("dve", lambda e, h=h, eq=eq, qe=qe, YS=YS: e.scalar_tensor_tensor(
                        out=qe.ap[:, h, :], in0=YS.ap[:, h, :], scalar=float(128 ** -0.5), in1=eq.ap, op0=ALU.mult,
                        op1=ALU.mult), (YS.b, eq.b), (qe.b,))
            yield
            for ti in tiles:
                vt = v_tok[(bn % 2) * 2 + ti]
                if d == 0:
                    for half in range(2):
                        bank = half

                        def vm(e, ti=ti, half=half, bank=bank, xb=xb):
                            for k in range(8):
                                i = e.matmul(psf(bank), lhsT=xb.ap[:, k, 2 + ti * 128:2 + (ti + 1) * 128],
                                             rhs=Wv.ap[:, k, half * 512:(half + 1) * 512], start=(k == 0), stop=(k == 7))
                            return i
                        S.op("pe", vm, (Wv.b, xb.b), (psbuf[bank],))
                        S.op("dve", lambda e, vt=vt, half=half, bank=bank: e.tensor_tensor(
                            out=vt.ap[:, half * 512:(half + 1) * 512], in0=psf(bank),
                            in1=bv_bc.ap[:, half * 512:(half + 1) * 512], op=ALU.add), (psbuf[bank], bv_bc.b), (vt.b,))
                    dma("sp", v_d[cslot, ti], vt.ap, lvd[((bn % 2) * 2 + ti)], (vt.b,), ())
                else:
                    dma("sp", vt.ap, v_d[cslot, ti], lvd[((bn % 2) * 2 + ti)], (), (vt.b,))
                if d == 1 and outf:
                    ogt = og[(bn % 2) * 2 + ti]
                    for half in range(2):
                        bank = half

                        def om_(e, ti=ti, half=half, bank=bank, xb=xb):
                            for k in range(8):
                                i = e.matmul(psf(bank, 64), lhsT=xb.ap[:, k, 2 + ti * 128:2 + ti * 128 + 64],
                                             rhs=Wo.ap[:, k, half * 512:(half + 1) * 512], start=(k == 0),
                                             stop=(k == 7))
                            return i
                        S.op("pe", om_, (Wo.b, xb.b), (psbuf[bank],))
                        S.op("dve", lambda e, half=half, bank=bank: e.tensor_tensor(
                            out=gtmp.ap[0:64, half * 512:(half + 1) * 512], in0=psf(bank, 64),
                            in1=bo_bc.ap[0:64, half * 512:(half + 1) * 512], op=ALU.add), (psbuf[bank], bo_bc.b),
                            (gtmp.b,))
                    S.op("act", lambda e, ogt=ogt: e.activation(out=ogt.ap[0:64, :], in_=gtmp.ap[0:64, :],
                                                                func=AF.Sigmoid), (gtmp.b,), (ogt.b,))
                yield

        def chain(bn, src, p0):
            outf = src == "c"
            qe, ke, Ee = qeT[bn % 2], keT[bn % 2], E_end[bn % 2]
            for ti in tiles:
                vt_n = cnt[0]
                cnt[0] += 1
                vt = v_tok[(bn % 2) * 2 + ti]
                if d == 1 and outf:
                    ogt = og[(bn % 2) * 2 + ti]
                if outf:
                    chunks = [(0, True), (64, False)] if d == 0 else [(64, False), (0, True)]
                else:
                    chunks = [(0, False), (64, False)] if d == 0 else [(64, False), (0, False)]
                ecol = {0: 2 * ti, 64: 2 * ti + 1}
                yield from chain_tile(C, d, qe, ke, slice(ti * 128, (ti + 1) * 128), vt, Ee, ecol, chunks,
                                      64 if outf else 0, with_den=True)
                if outf:
                    w = p0 // 128 + ti
                    S.op("act", lambda e: e.activation(out=dsm.ap[0:64, 0:4], in_=PS[0:64, 3, 192:196], func=AF.Copy),
                         (psbuf[3],), (dsm.b,))
                    S.op("act", lambda e: e.activation(out=dsm.ap[0:64, 4:8], in_=PS[0:64, 3, 320:324], func=AF.Copy),
                         (psbuf[3],), (dsm.b,))
                    S.op("dve", lambda e: e.tensor_tensor(out=den.ap[0:64, :], in0=dsm.ap[0:64, 0:4],
                                                          in1=dsm.ap[0:64, 4:8], op=ALU.add), (dsm.b,), (den.b,))
                    S.op("dve", lambda e: e.tensor_scalar(out=dsm.ap[0:64, 0:4], in0=den.ap[0:64, :], scalar1=-1.0,
                                                          scalar2=None, op0=ALU.mult), (den.b,), (dsm.b,))
                    S.op("dve", lambda e: e.tensor_tensor(out=den.ap[0:64, :], in0=den.ap[0:64, :],
                                                          in1=dsm.ap[0:64, 0:4], op=ALU.max), (den.b, dsm.b), (den.b,))
                    S.op("dve", lambda e: e.tensor_scalar(out=den.ap[0:64, :], in0=den.ap[0:64, :], scalar1=1.0,
                                                          scalar2=None, op0=ALU.max), (den.b,), (den.b,))
                    S.op("dve", lambda e: e.reciprocal(out=rden.ap[0:64, :], in_=den.ap[0:64, :]), (den.b,), (rden.b,))
                    ops = PS[0:64, 6:8, :].rearrange("p b (h v) -> p (b h) v", v=256)
                    S.op("dve", lambda e: e.tensor_tensor(
                        out=hout.ap[0:64, :].rearrange("p (h v) -> p h v", h=4), in0=ops,
                        in1=rden.ap[0:64, :].unsqueeze(2).broadcast_to([64, 4, 256]), op=ALU.mult),
                        (psbuf[6], psbuf[7], rden.b), (hout.b,))
                    if d == 0:
                        ht = h0s[vt_n % 2]
                        S.op("act", lambda e, ht=ht: e.activation(out=ht.ap[0:64, :], in_=hout.ap[0:64, :],
                                                                  func=AF.Copy), (hout.b,), (ht.b,))
                        dma("sp", o0_d[w * 64:(w + 1) * 64, :], ht.ap[0:64, :], lh[vt_n % 2], (ht.b,), ())
                    else:
                        ht = h0t[vt_n % 2]
                        dma("sp", ht.ap[0:64, :], o0_d[w * 64:(w + 1) * 64, :], lh[vt_n % 2], (), (ht.b,))
                        S.op("dve", lambda e, ht=ht: e.tensor_tensor(out=hout.ap[0:64, :], in0=hout.ap[0:64, :],
                                                                    in1=ht.ap[0:64, :], op=ALU.add),
                             (hout.b, ht.b), (hout.b,))
                        S.op("pool", lambda e: e.tensor_tensor(out=hsq.ap[0:64, :], in0=hout.ap[0:64, :],
                                                               in1=hout.ap[0:64, :], op=ALU.mult), (hout.b,),
                             (hsq.b,))
                        S.op("dve", lambda e: e.tensor_reduce(out=ss4.ap[0:64, :],
                                                              in_=hsq.ap[0:64, :].rearrange("p (h v) -> p h v", h=4),
                                                              axis=AX.X, op=ALU.add), (hsq.b,), (ss4.b,))
                        S.op("act", lambda e: e.activation(out=sd4.ap[0:64, :], in_=ss4.ap[0:64, :], func=AF.Sqrt,
                                                           bias=eps_c.ap[0:64, :], scale=1.0 / 256), (ss4.b, eps_c.b),
                             (sd4.b,))
                        S.op("dve", lambda e: e.reciprocal(out=rs4.ap[0:64, :], in_=sd4.ap[0:64, :]), (sd4.b,),
                             (rs4.b,))
                        S.op("dve", lambda e: e.tensor_tensor(
                            out=hout.ap[0:64, :].rearrange("p (h v) -> p h v", h=4),
                            in0=hout.ap[0:64, :].rearrange("p (h v) -> p h v", h=4),
                            in1=rs4.ap[0:64, :].unsqueeze(2).broadcast_to([64, 4, 256]), op=ALU.mult),
                            (hout.b, rs4.b), (hout.b,))
                        S.op("pool", lambda e: e.tensor_tensor(out=hsq.ap[0:64, :], in0=hout.ap[0:64, :],
                                                               in1=mln_bc.ap[0:64, :], op=ALU.mult),
                             (hout.b, mln_bc.b), (hsq.b,))
                        yb = ybt[vt_n % 2]
                        S.op("dve", lambda e, yb=yb, ogt=ogt: e.tensor_tensor(out=yb.ap[0:64, :], in0=hsq.ap[0:64, :],
                                                                             in1=ogt.ap[0:64, :], op=ALU.mult),
                             (hsq.b, ogt.b), (yb.b,))
                        dma("sp", yb_cols[w, 0:64, :], yb.ap[0:64, :], lyb[vt_n % 2], (yb.b,), ())
                yield

        pipeline([(prep(bn, *blk), chain(bn, *blk)) for bn, blk in enumerate(blocks)])
        S.barrier()
        A.reset(m0)

    def phase_merge():
        m0 = A.mark()
        Wpa = A.alloc((8, D), BF16)
        Wpb = A.alloc((8, D), BF16)
        Wo = A.alloc((8, D), BF16)
        Wga = A.alloc((8, D), BF16)
        Wgb = A.alloc((8, D), BF16)
        lws = [S.new_lane(f"gw{i}") for i in range(5)]
        for t, src, ln in ((Wpa, w_pa, lws[0]), (Wpb, w_pb, lws[1]), (Wo, w_out, lws[2])):
            dma("pool", t.ap, src.rearrange("(k p) n -> p k n", p=128), ln, (), (t.b,))
        load_w(Wga, O_GA, D, lws[3])
        load_w(Wgb, O_GB, D, lws[4])
        Wrt = A.alloc((8, 36), F32)
        dma_small(Wrt.ap, w_rt.rearrange("(k p) n -> p k n", p=128), (), (Wrt.b,))
        bga_bc = A.alloc((D,), F32)
        bgb_bc = A.alloc((D,), F32)
        g1_bc = A.alloc((D,), F32)
        G2_bc = A.alloc((D,), F32)
        SH2_bc = A.alloc((D,), F32)
        brt_bc = A.alloc((36,), F32)
        bc_load_in(bga_bc, b_in[0:1, O_GA:O_GA + D], D)
        bc_load_in(bgb_bc, b_in[0:1, O_GB:O_GB + D], D)
        bc_load(g1_bc, 6)
        bc_load(G2_bc, 4)
        bc_load(SH2_bc, 5)
        bc_load_in(brt_bc, b_rt[0:1, :], 36)
        S.barrier()
        yat = [A.alloc((D,), BF16) for _ in range(2)]
        ybt = [A.alloc((D,), BF16) for _ in range(2)]
        xt = [A.alloc((D,), F32) for _ in range(2)]
        xnt = [A.alloc((8, 128), BF16) for _ in range(2)]
        ll = [[S.new_lane(f"gl{j}{i}") for i in range(2)] for j in range(4)]
        yaT = A.alloc((8, 128), BF16)
        ybT = A.alloc((8, 128), BF16)
        yT = A.alloc((8, 128), BF16)
        sga = A.alloc((512,), F32)
        sgb = A.alloc((512,), F32)
        yf = A.alloc((D,), F32)
        t2 = A.alloc((512,), F32)
        ybf2 = [A.alloc((D,), BF16) for _ in range(2)]
        yf2 = A.alloc((D,), F32)
        xm = [A.alloc((D,), F32) for _ in range(2)]
        lxm = [S.new_lane(f"gxm{i}") for i in range(2)]
        junk = A.alloc((D,), BF16)
        ss = A.alloc((1,), F32)
        sd = A.alloc((1,), F32)
        rs = A.alloc((1,), F32)
        h2 = A.alloc((D,), F32)
        h2T2 = [A.alloc((8, 128), F32) for _ in range(2)]
        h2st = [A.alloc((8, 512), BF16) for _ in range(2)]
        lh2 = [S.new_lane(f"gh2{i}") for i in range(2)]
        lg = A.alloc((36,), F32)
        sm = A.alloc((64,), F32)
        elm = A.alloc((32,), F32)
        oh1 = A.alloc((32,), F32)
        oh2 = A.alloc((32,), F32)
        top8 = A.alloc((8,), F32)

        def dv(fn, r, w):
            S.op("dve", fn, r, w)
        def trp(e, src, bank):
            pv = psb(bank).rearrange("p (k t) -> p k t", k=8)
            for k in range(8):
                ins = e.transpose(out=pv[:, k, :], in_=src.ap[:, k * 128:(k + 1) * 128], identity=ident_b.ap)
            return ins

        def mmw(e, lhs, W, half, bank):
            for k in range(8):
                ins = e.matmul(psf(bank), lhsT=lhs.ap[:, k, :], rhs=W.ap[:, k, half * 512:(half + 1) * 512],
                               start=(k == 0), stop=(k == 7))
            return ins

        def sA(i):
            a = i % 2
            ya, yb, x, xn = yat[a], ybt[a], xt[a], xnt[a]
            rows = slice(i * 128, (i + 1) * 128)
            ybf = ybf2[a]
            dma("sp", ya.ap, ya_d[rows, :], ll[0][a], (), (ya.b,))
            dma("sp", yb.ap, yb_d[rows, :], ll[1][a], (), (yb.b,))
            dma("sp", x.ap, xv[rows, :], ll[2][a], (), (x.b,))
            dma("sp", xn.ap, xnT_r[:, :, i * 128:(i + 1) * 128], ll[3][a], (), (xn.b,))
            S.op("pe", lambda e, ya=ya: trp(e, ya, 0), (ya.b, ident_b.b), (psbuf[0],))
            S.op("act", lambda e: e.activation(out=yaT.ap, in_=psb(0).rearrange("p (k t) -> p k t", k=8), func=AF.Copy),
                 (psbuf[0],), (yaT.b,))
            S.op("pe", lambda e, yb=yb: trp(e, yb, 1), (yb.b, ident_b.b), (psbuf[1],))
            S.op("dve", lambda e: e.tensor_copy(out=ybT.ap, in_=psb(1).rearrange("p (k t) -> p k t", k=8)),
                 (psbuf[1],), (ybT.b,))
            for half in range(2):
                hs = slice(half * 512, (half + 1) * 512)
                S.op("pe", lambda e, xn=xn, half=half: mmw(e, xn, Wga, half, 2), (xn.b, Wga.b), (psbuf[2],))
                dv(lambda e, hs=hs: e.tensor_tensor(out=t2.ap, in0=psf(2), in1=bga_bc.ap[:, hs], op=ALU.add),
                   (psbuf[2], bga_bc.b), (t2.b,))
                S.op("act", lambda e: e.activation(out=sga.ap, in_=t2.ap, func=AF.Sigmoid), (t2.b,), (sga.b,))
                S.op("pe", lambda e, half=half: mmw(e, yaT, Wpa, half, 3), (yaT.b, Wpa.b), (psbuf[3],))
                dv(lambda e, hs=hs: e.tensor_tensor(out=yf.ap[:, hs], in0=psf(3), in1=sga.ap, op=ALU.mult),
                   (psbuf[3], sga.b), (yf.b,))
                S.op("pe", lambda e, xn=xn, half=half: mmw(e, xn, Wgb, half, 2), (xn.b, Wgb.b), (psbuf[2],))
                dv(lambda e, hs=hs: e.tensor_tensor(out=t2.ap, in0=psf(2), in1=bgb_bc.ap[:, hs], op=ALU.add),
                   (psbuf[2], bgb_bc.b), (t2.b,))
                S.op("act", lambda e: e.activation(out=sgb.ap, in_=t2.ap, func=AF.Sigmoid), (t2.b,), (sgb.b,))
                S.op("pe", lambda e, half=half: mmw(e, ybT, Wpb, half, 3), (ybT.b, Wpb.b), (psbuf[3],))
                dv(lambda e: e.tensor_tensor(out=t2.ap, in0=psf(3), in1=sgb.ap, op=ALU.mult), (psbuf[3], sgb.b),
                   (t2.b,))
                S.op("pool", lambda e, hs=hs: e.tensor_tensor(out=ybf.ap[:, hs], in0=yf.ap[:, hs], in1=t2.ap,
                                                              op=ALU.add), (yf.b, t2.b), (ybf.b,))

        def sB(i):
            a = i % 2
            x = xt[a]
            rows = slice(i * 128, (i + 1) * 128)
            ybf = ybf2[a]
            h2T = h2T2[a]
            S.op("pe", lambda e, ybf=ybf: trp(e, ybf, 4), (ybf.b, ident_b.b), (psbuf[4],))
            S.op("act", lambda e: e.activation(out=yT.ap, in_=psb(4).rearrange("p (k t) -> p k t", k=8), func=AF.Copy),
                 (psbuf[4],), (yT.b,))
            xmt = xm[a]
            for half in range(2):
                hs = slice(half * 512, (half + 1) * 512)
                S.op("pe", lambda e, half=half: mmw(e, yT, Wo, half, 5), (yT.b, Wo.b), (psbuf[5],))
                dv(lambda e, hs=hs, half=half: e.tensor_tensor(out=yf2.ap[:, hs], in0=psf(5), in1=g1_bc.ap[:, hs],
                                                               op=ALU.mult), (psbuf[5], g1_bc.b), (yf2.b,))
                S.op("pool", lambda e, hs=hs, xmt=xmt, x=x: e.tensor_tensor(out=xmt.ap[:, hs], in0=yf2.ap[:, hs],
                                                                           in1=x.ap[:, hs], op=ALU.add),
                     (yf2.b, x.b), (xmt.b,))
            dma("sp", xmid_d[rows, :], xmt.ap, lxm[a], (xmt.b,), ())
            S.op("act", lambda e, xmt=xmt: e.activation(out=junk.ap, in_=xmt.ap, func=AF.Square, accum_out=ss.ap),
                 (xmt.b,), (junk.b, ss.b))
            rstd_ops(ss, sd, rs, D)
            dv(lambda e, xmt=xmt: e.scalar_tensor_tensor(out=h2.ap, in0=xmt.ap, scalar=rs.ap, in1=G2_bc.ap,
                                                         op0=ALU.mult, op1=ALU.mult), (xmt.b, rs.b, G2_bc.b), (h2.b,))
            S.op("pool", lambda e: e.tensor_tensor(out=h2.ap, in0=h2.ap, in1=SH2_bc.ap, op=ALU.add),
                 (h2.b, SH2_bc.b), (h2.b,))

            def trf(e):
                pv = PS[:, 6:8, :].rearrange("p b (k t) -> p (b k) t", t=128)
                for k in range(8):
                    ins = e.transpose(out=pv[:, k, :], in_=h2.ap[:, k * 128:(k + 1) * 128], identity=ident_f.ap)
                return ins
            S.op("pe", trf, (h2.b, ident_f.b), (psbuf[6], psbuf[7]))
            S.op("act", lambda e: e.activation(out=h2T.ap, in_=PS[:, 6:8, :].rearrange("p b (k t) -> p (b k) t", t=128),
                                               func=AF.Copy), (psbuf[6], psbuf[7]), (h2T.b,))
            st = h2st[(i // 4) % 2]
            S.op("pool", lambda e, st=st, c=i % 4: e.tensor_copy(out=st.ap[:, :, c * 128:(c + 1) * 128], in_=h2T.ap),
                 (h2T.b,), (st.b,))
            if i % 4 == 3:
                dma("sp", h2T_d[:, :, (i - 3) * 128:(i + 1) * 128], st.ap, lh2[(i // 4) % 2], (st.b,), ())


        def sC(i):
            a = i % 2
            h2T = h2T2[a]
            def lgm(e):
                for k in range(8):
                    ins = e.matmul(psf(4, 128, 36), lhsT=h2T.ap[:, k, :], rhs=Wrt.ap[:, k, :], start=(k == 0),
                                   stop=(k == 7))
                return ins
            S.op("pe", lgm, (h2T.b, Wrt.b), (psbuf[4],))
            gl = lg.ap[:, 0:4]
            el = lg.ap[:, 4:36]
            gmax, ngmax, gsum, gw = (sm.ap[:, j:j + 1] for j in range(4))
            goh = sm.ap[:, 4:8]
            gexp = sm.ap[:, 8:12]
            pen = sm.ap[:, 12:16]
            dvv, ed, w1, w2, w1g, w2g = (sm.ap[:, j:j + 1] for j in range(16, 22))
            dv(lambda e: e.tensor_tensor(out=lg.ap, in0=psf(4, 128, 36), in1=brt_bc.ap, op=ALU.add),
               (psbuf[4], brt_bc.b), (lg.b,))
            dv(lambda e: e.tensor_reduce(out=gmax, in_=gl, axis=AX.X, op=ALU.max), (lg.b,), (sm.b,))
            dv(lambda e: e.tensor_scalar(out=goh, in0=gl, scalar1=gmax, scalar2=None, op0=ALU.is_equal),
               (lg.b, sm.b), (sm.b,))
            dv(lambda e: e.tensor_scalar(out=ngmax, in0=gmax, scalar1=-1.0, scalar2=None, op0=ALU.mult), (sm.b,),
               (sm.b,))
            S.op("act", lambda e: e.activation(out=gexp, in_=gl, func=AF.Exp, bias=ngmax, accum_out=gsum),
                 (lg.b, sm.b), (sm.b,))
            dv(lambda e: e.reciprocal(out=gw, in_=gsum), (sm.b,), (sm.b,))
            dv(lambda e: e.tensor_scalar(out=pen, in0=goh, scalar1=-1.0, scalar2=1e30, op0=ALU.add, op1=ALU.mult),
               (sm.b,), (sm.b,))
            dv(lambda e: e.tensor_tensor(out=elm.ap.rearrange("p (g x) -> p g x", g=4),
                                         in0=el.rearrange("p (g x) -> p g x", g=4),
                                         in1=pen.unsqueeze(2).broadcast_to([128, 4, 8]), op=ALU.add),
               (lg.b, sm.b), (elm.b,))
            dv(lambda e: e.max(out=top8.ap, in_=elm.ap), (elm.b,), (top8.b,))
            dv(lambda e: e.tensor_scalar(out=oh1.ap, in0=elm.ap, scalar1=top8.ap[:, 0:1], scalar2=None,
                                         op0=ALU.is_equal), (elm.b, top8.b), (oh1.b,))
            dv(lambda e: e.tensor_scalar(out=oh2.ap, in0=elm.ap, scalar1=top8.ap[:, 1:2], scalar2=None,
                                         op0=ALU.is_equal), (elm.b, top8.b), (oh2.b,))
            dv(lambda e: e.tensor_tensor(out=dvv, in0=top8.ap[:, 1:2], in1=top8.ap[:, 0:1], op=ALU.subtract),
               (top8.b,), (sm.b,))
            S.op("act", lambda e: e.activation(out=ed, in_=dvv, func=AF.Exp), (sm.b,), (sm.b,))
            dv(lambda e: e.tensor_scalar(out=ed, in0=ed, scalar1=1.0, scalar2=None, op0=ALU.add), (sm.b,), (sm.b,))
            dv(lambda e: e.reciprocal(out=w1, in_=ed), (sm.b,), (sm.b,))
            dv(lambda e: e.tensor_scalar(out=w2, in0=w1, scalar1=-1.0, scalar2=1.0, op0=ALU.mult, op1=ALU.add),
               (sm.b,), (sm.b,))
            dv(lambda e: e.tensor_tensor(out=w1g, in0=w1, in1=gw, op=ALU.mult), (sm.b,), (sm.b,))
            dv(lambda e: e.tensor_tensor(out=w2g, in0=w2, in1=gw, op=ALU.mult), (sm.b,), (sm.b,))
            dv(lambda e: e.tensor_scalar(out=oh1.ap, in0=oh1.ap, scalar1=w1g, scalar2=None, op0=ALU.mult),
               (oh1.b, sm.b), (oh1.b,))
            dv(lambda e, i=i: e.scalar_tensor_tensor(out=comb_all.ap[:, i, :], in0=oh2.ap, scalar=w2g, in1=oh1.ap,
                                                     op0=ALU.mult, op1=ALU.add), (oh2.b, oh1.b, sm.b), (comb_all.b,))

        pipeline_k([sA, sB, sC], list(range(TO // 128)))
        if debug:
            dbg_comb = nc.dram_tensor("dbg_comb", [128, 32, 32], F32, kind="ExternalOutput").ap()
            dma_small(dbg_comb, comb_all.ap, (comb_all.b,), ())
        S.barrier()
        A.reset(m0)

    def phase_moe():
        m0 = A.mark()
        HT = TO // 2
        h2h = A.alloc((8, HT), BF16)
        acc = A.alloc((HT // 128, D), F32)
        lh = S.new_lane("moe_h2")
        Wg = [A.alloc((8, DE), BF16) for _ in range(2)]
        Wu = [A.alloc((8, DE), BF16) for _ in range(2)]
        Wd = [A.alloc((4, D), BF16) for _ in range(2)]
        lw = [[S.new_lane(f"moew{j}{i}") for i in range(2)] for j in range(3)]
        sg = [A.alloc((512,), BF16) for _ in range(2)]
        h1T = [A.alloc((4, 512), BF16) for _ in range(2)]
        g2_bc = A.alloc((D,), F32)
        fin_bc = A.alloc((D,), F32)
        bc_load(g2_bc, 7)
        bc_load_in(fin_bc, fing[0:1, :], D)
        S.barrier()
        xmt = [A.alloc((D,), F32) for _ in range(2)]
        lxm = [S.new_lane(f"moex{i}") for i in range(2)]
        ot = [A.alloc((D,), F32) for _ in range(2)]
        lot = [S.new_lane(f"moeo{i}") for i in range(2)]
        tt = A.alloc((D,), F32)
        tts = [tt, tt]
        junk = A.alloc((D,), BF16)
        ssf = [A.alloc((1,), F32) for _ in range(2)]
        sdf = [A.alloc((1,), F32) for _ in range(2)]
        rsf = [A.alloc((1,), F32) for _ in range(2)]
        accb = [Buf(f"acc{i}") for i in range(HT // 128)]
        fcnt = [0]

        def final_tile(hf, tl):
            gt = hf * (HT // 128) + tl
            a = fcnt[0] % 2
            fcnt[0] += 1
            xm = xmt[a]
            o = ot[a]
            t_ = tts[a]
            rows = slice(gt * 128, (gt + 1) * 128)
            dma("sp", xm.ap, xmid_d[rows, :], lxm[a], (), (xm.b,))
            S.op("dve", lambda e: e.tensor_tensor(out=t_.ap, in0=acc.ap[:, tl, :], in1=g2_bc.ap, op=ALU.mult),
                 (accb[tl], g2_bc.b), (t_.b,))
            S.op("pool", lambda e: e.tensor_tensor(out=t_.ap, in0=t_.ap, in1=xm.ap, op=ALU.add),
                 (t_.b, xm.b), (t_.b,))
            S.op("act", lambda e: e.activation(out=junk.ap, in_=t_.ap, func=AF.Square, accum_out=ssf[a].ap), (t_.b,),
                 (junk.b, ssf[a].b))
            rstd_ops(ssf[a], sdf[a], rsf[a], D)
            S.op("dve", lambda e: e.scalar_tensor_tensor(out=o.ap, in0=t_.ap, scalar=rsf[a].ap, in1=fin_bc.ap,
                                                         op0=ALU.mult, op1=ALU.mult),
                 (t_.b, rsf[a].b, fin_bc.b), (o.b,))
            out_dmas.append(dma("sp", out[rows, :], o.ap, lot[a], (o.b,), ()))

        out_dmas = []
        nw = 0
        for hf in range(2):
            dma("sp", h2h.ap, h2T_d[:, :, hf * HT:(hf + 1) * HT], lh, (), (h2h.b,))
            for ex in range(NE):
                a = nw % 2
                nw += 1
                wg, wu, wd = Wg[a], Wu[a], Wd[a]
                dma("pool", wg.ap, w_gate[ex].rearrange("(k p) n -> p k n", p=128), lw[0][a], (), (wg.b,))
                dma("pool", wu.ap, w_up[ex].rearrange("(k p) n -> p k n", p=128), lw[1][a], (), (wu.b,))
                dma("pool", wd.ap, w_down[ex].rearrange("(k p) n -> p k n", p=128), lw[2][a], (), (wd.b,))
                for blk in range(HT // 512):
                    h1 = h1T[blk % 2]
                    ts = slice(blk * 512, (blk + 1) * 512)
                    for jc in range(4):
                        bg, bu = (0, 2) if jc % 2 == 0 else (1, 3)
                        sgt = sg[jc % 2]

                        def gmm(e, W, bank, jc=jc, ts=ts):
                            for k in range(8):
                                ins = e.matmul(psf(bank), lhsT=W.ap[:, k, jc * 128:(jc + 1) * 128], rhs=h2h.ap[:, k, ts],
                                               start=(k == 0), stop=(k == 7))
                            return ins
                        S.op("pe", lambda e, wg=wg, bg=bg, f=gmm: f(e, wg, bg), (wg.b, h2h.b), (psbuf[bg],))
                        S.op("act", lambda e, sgt=sgt, bg=bg: e.activation(out=sgt.ap, in_=psf(bg), func=AF.Silu),
                             (psbuf[bg],), (sgt.b,))
                        S.op("pe", lambda e, wu=wu, bu=bu, f=gmm: f(e, wu, bu), (wu.b, h2h.b), (psbuf[bu],))
                        S.op("dve", lambda e, sgt=sgt, bu=bu, h1=h1, jc=jc: e.tensor_tensor(
                            out=h1.ap[:, jc, :], in0=psf(bu), in1=sgt.ap, op=ALU.mult), (psbuf[bu], sgt.b), (h1.b,))
                    for t4 in range(4):
                        tl = blk * 4 + t4
                        gt = hf * (HT // 128) + tl
                        for dh in range(2):
                            bank = 4 + (t4 * 2 + dh) % 4

                            def dmm(e, bank=bank, t4=t4, dh=dh, h1=h1, wd=wd):
                                for jc in range(4):
                                    ins = e.matmul(psf(bank), lhsT=h1.ap[:, jc, t4 * 128:(t4 + 1) * 128],
                                                   rhs=wd.ap[:, jc, dh * 512:(dh + 1) * 512], start=(jc == 0),
                                                   stop=(jc == 3))
                                return ins
                            S.op("pe", dmm, (h1.b, wd.b), (psbuf[bank],))
                            accv = acc.ap[:, tl, dh * 512:(dh + 1) * 512]
                            cs = comb_all.ap[:, gt, ex:ex + 1]
                            if ex == 0:
                                S.op("dve", lambda e, accv=accv, cs=cs, bank=bank: e.tensor_scalar(
                                    out=accv, in0=psf(bank), scalar1=cs, scalar2=None, op0=ALU.mult),
                                    (psbuf[bank], comb_all.b), (accb[tl],))
                            else:
                                S.op("dve", lambda e, accv=accv, cs=cs, bank=bank: e.scalar_tensor_tensor(
                                    out=accv, in0=psf(bank), scalar=cs, in1=accv, op0=ALU.mult, op1=ALU.add),
                                    (psbuf[bank], comb_all.b, accb[tl]), (accb[tl],))
                        if ex == NE - 1:
                            final_tile(hf, tl)
        A.reset(m0)
        return out_dmas

    order = ["mod", "norm", "hgrn", "mlstm", "merge", "moe"]
    upto = order.index(stop_after) if stop_after is not None else len(order) - 1
    phase_mod()
    A.reset(base_mark)
    finals = []
    if upto >= 1:
        phase_norm()
        A.reset(base_mark)
    if upto >= 2:
        phase_hgrn(0)
        phase_hgrn(1)
    if upto >= 3:
        phase_mlstm(0)
        phase_mlstm(1)
    if upto >= 4:
        phase_merge()
    if upto >= 5:
        finals = phase_moe()
    else:
        S.barrier()
        fin = A.alloc((D,), F32)
        memset("pool", fin, fin.ap, 0.0)
        finals = [dma("sp", out[0:128, :], fin.ap, L_setup, (fin.b,), ())]
    lastper = {}
    for o_ in finals:
        lastper[id(o_.lane)] = o_
    with nc.Block() as block:
        S.emit(block, eng_sems, final_waits=list(lastper.values()))
    return nc


def _core_inputs(inp, core):
    b = core // 2
    flip = core % 2 == 1
    f32 = np.float32
    x = np.asarray(inp["x"][b], f32)
    ctx = np.asarray(inp["ctx"][b], f32)
    w_in = np.asarray(inp["w_in"][0], f32)
    b_in = np.asarray(inp["b_in"][0], f32)
    lbl = np.asarray(inp["hg_lb_logits"], f32)
    convw = np.asarray(inp["ml_conv_w"][0], f32)
    if flip:
        x = x[::-1]
        ctx = ctx[::-1]
        perm = np.arange(DIN)
        perm[O_HF0:O_HF0 + D] = np.arange(O_HF1, O_HF1 + D)
        perm[O_HF1:O_HF1 + D] = np.arange(O_HF0, O_HF0 + D)
        perm[O_MI:O_MI + 4] = np.arange(O_MI + 4, O_MI + 8)
        perm[O_MI + 4:O_MI + 8] = np.arange(O_MI, O_MI + 4)
        perm[O_MF:O_MF + 4] = np.arange(O_MF + 4, O_MF + 8)
        perm[O_MF + 4:O_MF + 8] = np.arange(O_MF, O_MF + 4)
        w_in = w_in[:, perm]
        b_in = b_in[perm]
        lbl = lbl[::-1]
        convw = convw[::-1]
    cv = np.concatenate([np.asarray(inp["c"][b], f32).reshape(8, 128).T,
                         np.asarray(inp["c_ctx"], f32).reshape(8, 128).T], axis=1)
    m = {
        "xv": np.ascontiguousarray(x), "ctxv": np.ascontiguousarray(ctx), "cvec": np.ascontiguousarray(cv),
        "w_ada": np.asarray(inp["w_ada"][0], f32), "b_ada": np.asarray(inp["b_ada"], f32).reshape(1, -1),
        "n1g": np.asarray(inp["norm1_g"], f32).reshape(1, -1), "n2g": np.asarray(inp["norm2_g"], f32).reshape(1, -1),
        "fing": np.asarray(inp["final_norm_g"], f32).reshape(1, -1),
        "w_in": np.ascontiguousarray(w_in), "b_in": np.ascontiguousarray(b_in).reshape(1, -1),
        "lbl": np.ascontiguousarray(lbl.reshape(4, D)),
        "hgn": np.asarray(inp["hg_norm_g"], f32).reshape(1, -1), "mln": np.asarray(inp["ml_norm_g"], f32).reshape(1, -1),
        "convw": np.ascontiguousarray(convw), "convb": np.asarray(inp["ml_conv_b"], f32).reshape(1, -1),
        "w_pa": np.asarray(inp["w_branch_a"][0], f32), "w_pb": np.asarray(inp["w_branch_b"][0], f32),
        "w_out": np.asarray(inp["w_out"][0], f32),
        "w_rt": np.ascontiguousarray(np.concatenate([np.asarray(inp["w_group"][0], f32),
                                                     np.asarray(inp["w_router"][0], f32)], axis=1)),
        "b_rt": np.concatenate([np.asarray(inp["b_group"][0], f32), np.asarray(inp["b_router"][0], f32)]).reshape(1, -1),
        "w_gate": np.asarray(inp["w_gate"][0], f32), "w_up": np.asarray(inp["w_up"][0], f32),
        "w_down": np.asarray(inp["w_down"][0], f32),
    }
    return m


_NC_CACHE = {}


def kernel(**inputs):
    if "nc" not in _NC_CACHE:
        _NC_CACHE["nc"] = build_program()
    nc = _NC_CACHE["nc"]
    in_maps = [_core_inputs(inputs, c) for c in range(8)]
    res = run_bass_kernel_spmd(nc, in_maps, core_ids=list(range(8)))
    outp = np.empty((4, T, D), np.float32)
    for c in range(8):
        o = np.asarray(res.results[c]["out"], np.float32)
        b = c // 2
        if c % 2 == 0:
            outp[b, 0:TO] = o
        else:
            outp[b, TO:T] = o[::-1]
    return outp
```
